# Optimizing a Trainium2 kernel written in Bass

```python
import jax, jax.numpy as jnp
from jax import lax
import numpy as np

D_MODEL = 1024
BATCH = 32
SEQ = 2048
DEPTH = 4

HEAD_DIM = 64
MIX_WIDTH = D_MODEL
SB_WIDTH = MIX_WIDTH // 2
SB_HEADS = SB_WIDTH // HEAD_DIM
SWA_WIDTH = MIX_WIDTH - SB_WIDTH
SWA_Q_HEADS = SWA_WIDTH // HEAD_DIM
SWA_GROUP = 4
SWA_KV_HEADS = SWA_Q_HEADS // SWA_GROUP
SWA_KV_WIDTH = SWA_KV_HEADS * HEAD_DIM
IN_COLS = 3 * SB_WIDTH + SWA_WIDTH + 2 * SWA_KV_WIDTH
WINDOW = 128
QBLK = 128
N_BUCKETS = 32
MAX_DISTANCE = 128
N_MEM = 256
MEM_HEADS = 4
MEM_HEAD_DIM = 128
MEM_WIDTH = MEM_HEADS * MEM_HEAD_DIM
D_FF = (7 * D_MODEL) // 2
N_EXPERTS = 8
TOP_K = 2
MOE_BLK = 256
EPS = 1e-6
N_DENSE = (DEPTH + 1) // 2
N_MOE = DEPTH // 2

kernel_name = "hymba_stickbreak_swa_sink_memxattn_moe"


def rms_norm(x, g):
    xf = x.astype(jnp.float32)
    y = xf * lax.rsqrt(jnp.mean(xf * xf, axis=-1, keepdims=True) + EPS)
    return (y * g.astype(jnp.float32)).astype(x.dtype)


def t5_buckets(dist):
    n = np.maximum(dist, 0)
    max_exact = N_BUCKETS // 2
    large = max_exact + (np.log(np.maximum(n, 1) / max_exact) / np.log(MAX_DISTANCE / max_exact)
                         * (N_BUCKETS - max_exact)).astype(np.int32)
    large = np.minimum(large, N_BUCKETS - 1)
    return np.where(n < max_exact, n, large).astype(np.int32)


def stick_breaking_attention(q, k, v):
    S, d = q.shape[2], q.shape[3]
    scale = d ** -0.5
    outs = []
    for i in range(S // QBLK):
        t0, t1 = i * QBLK, (i + 1) * QBLK
        qb, kp, vp = q[:, :, t0:t1], k[:, :, :t1], v[:, :, :t1]
        z = jnp.einsum('bhqd,bhkd->bhqk', qb, kp).astype(jnp.float32) * scale
        strict = (jnp.arange(t1)[None, :] < (t0 + jnp.arange(QBLK))[:, None])
        log_beta = jax.nn.log_sigmoid(z)
        log_one_minus = jnp.where(strict, jax.nn.log_sigmoid(-z), 0.0)
        tail = lax.cumsum(log_one_minus, axis=3, reverse=True) - log_one_minus
        a = jnp.where(strict, jnp.exp(log_beta + tail), 0.0)
        outs.append(jnp.einsum('bhqk,bhkd->bhqd', a.astype(v.dtype), vp))
    return jnp.concatenate(outs, axis=2)


def sliding_window_sink_attention(q, k, v, sinks, bias):
    B, S, _, d = q.shape
    nb = S // WINDOW
    qb = q.reshape(B, nb, WINDOW, SWA_KV_HEADS, SWA_GROUP, d)

    def band(t):
        tb = t.reshape(B, nb, WINDOW, SWA_KV_HEADS, d)
        prev = jnp.pad(tb, ((0, 0), (1, 0), (0, 0), (0, 0), (0, 0)))[:, :-1]
        return jnp.concatenate([prev, tb], axis=2)

    kw, vw = band(k), band(v)
    s = jnp.einsum('bnqhgd,bnkhd->bnhgqk', qb, kw).astype(jnp.float32) * (d ** -0.5)
    s = s + bias.astype(jnp.float32).reshape(SWA_KV_HEADS, SWA_GROUP, WINDOW, 2 * WINDOW)
    blk = jnp.arange(nb)[:, None, None]
    tpos = blk * WINDOW + jnp.arange(WINDOW)[None, :, None]
    spos = (blk - 1) * WINDOW + jnp.arange(2 * WINDOW)[None, None, :]
    valid = (spos <= tpos) & (tpos - spos < WINDOW) & (spos >= 0)
    s = jnp.where(valid[None, :, None, None], s, -jnp.inf)
    sink = sinks.astype(jnp.float32).reshape(1, 1, SWA_KV_HEADS, SWA_GROUP, 1, 1)
    m = jnp.maximum(jnp.max(s, axis=-1, keepdims=True), sink)
    p = jnp.exp(s - m)
    p = p / (jnp.sum(p, axis=-1, keepdims=True) + jnp.exp(sink - m))
    o = jnp.einsum('bnhgqk,bnkhd->bnqhgd', p.astype(v.dtype), vw)
    return o.reshape(B, S, SWA_Q_HEADS * d)


def memory_cross_attention(h, m, wq, wkv, q_gain, k_gain):
    B, S, _ = h.shape
    N = m.shape[1]
    q = (h @ wq).reshape(B, S, MEM_HEADS, MEM_HEAD_DIM)
    kv = m @ wkv
    k = kv[..., :MEM_WIDTH].reshape(B, N, MEM_HEADS, MEM_HEAD_DIM)
    v = kv[..., MEM_WIDTH:].reshape(B, N, MEM_HEADS, MEM_HEAD_DIM)
    q, k = rms_norm(q, q_gain), rms_norm(k, k_gain)
    s = jnp.einsum('bshd,bnhd->bhsn', q, k).astype(jnp.float32) * (MEM_HEAD_DIM ** -0.5)
    p = jax.nn.softmax(s, axis=-1)
    o = jnp.einsum('bhsn,bnhd->bshd', p.astype(v.dtype), v)
    return o.reshape(B, S, MEM_WIDTH)


def swiglu(h, w_gate, w_up, w_down):
    return (jax.nn.silu(h @ w_gate) * (h @ w_up)) @ w_down


def moe_swiglu(h, router_w, router_b, w_gate, w_up, w_down):
    T, D = h.shape
    logits = (h @ router_w).astype(jnp.float32) + router_b.astype(jnp.float32)
    top_val, top_idx = lax.top_k(logits, TOP_K)
    gates = jax.nn.softmax(top_val, axis=-1)
    flat_e = top_idx.reshape(-1)
    flat_t = jnp.repeat(jnp.arange(T, dtype=jnp.int32), TOP_K)
    flat_g = gates.reshape(-1).astype(h.dtype)
    onehot = jax.nn.one_hot(flat_e, N_EXPERTS, dtype=jnp.int32)
    rank = jnp.sum(jnp.cumsum(onehot, axis=0) * onehot, axis=-1) - 1
    counts = jnp.sum(onehot, axis=0)
    pcounts = (counts + MOE_BLK - 1) // MOE_BLK * MOE_BLK
    pends = jnp.cumsum(pcounts)
    pstarts = pends - pcounts
    dest = pstarts[flat_e] + rank
    P = T * TOP_K + N_EXPERTS * MOE_BLK
    nb = P // MOE_BLK
    row_tok = jnp.zeros((P,), jnp.int32).at[dest].set(flat_t)
    row_gate = jnp.zeros((P,), h.dtype).at[dest].set(flat_g)
    blk_e = jnp.minimum(jnp.searchsorted(pends, jnp.arange(nb) * MOE_BLK, side='right'), N_EXPERTS - 1)

    def one_block(args):
        tok, e = args
        return swiglu(h[tok], w_gate[e], w_up[e], w_down[e])

    ys = lax.map(one_block, (row_tok.reshape(nb, MOE_BLK), blk_e)).reshape(P, D)
    return jax.ops.segment_sum(ys * row_gate[:, None], row_tok, num_segments=T)


def setup_inputs(seed: int = 0) -> dict:
    key = jax.random.key(seed)
    ks = iter(jax.random.split(key, 40))
    f32 = jnp.float32
    res_scale = (2 * DEPTH) ** -0.5

    def w(shape, fan_in, extra=1.0):
        return jax.random.normal(next(ks), shape, f32) * (fan_in ** -0.5) * extra

    def gain(shape):
        return 1.0 + 0.02 * jax.random.normal(next(ks), shape, f32)

    return {
        "x": jax.random.normal(next(ks), (BATCH, SEQ, D_MODEL), f32),
        "mem": jax.random.normal(next(ks), (BATCH, N_MEM, D_MODEL), f32),
        "norm_mix": gain((DEPTH, D_MODEL)),
        "w_in": w((DEPTH, D_MODEL, IN_COLS), D_MODEL),
        "sb_out_gain": gain((DEPTH, SB_WIDTH)),
        "swa_q_gain": gain((DEPTH, HEAD_DIM)),
        "swa_k_gain": gain((DEPTH, HEAD_DIM)),
        "swa_sinks": 0.5 * jax.random.normal(next(ks), (DEPTH, SWA_Q_HEADS), f32),
        "swa_out_gain": gain((DEPTH, SWA_WIDTH)),
        "rel_bias": 0.5 * jax.random.normal(next(ks), (N_BUCKETS, SWA_Q_HEADS), f32),
        "w_out": w((DEPTH, MIX_WIDTH, D_MODEL), MIX_WIDTH, res_scale),
        "norm_xattn": gain((DEPTH, D_MODEL)),
        "norm_mem": gain((DEPTH, D_MODEL)),
        "xattn_wq": w((DEPTH, D_MODEL, MEM_WIDTH), D_MODEL),
        "xattn_wkv": w((DEPTH, D_MODEL, 2 * MEM_WIDTH), D_MODEL),
        "xattn_q_gain": gain((DEPTH, MEM_HEAD_DIM)),
        "xattn_k_gain": gain((DEPTH, MEM_HEAD_DIM)),
        "xattn_wo": w((DEPTH, MEM_WIDTH, D_MODEL), MEM_WIDTH, res_scale),
        "norm_ffn": gain((DEPTH, D_MODEL)),
        "dense_w_gate": w((N_DENSE, D_MODEL, D_FF), D_MODEL),
        "dense_w_up": w((N_DENSE, D_MODEL, D_FF), D_MODEL),
        "dense_w_down": w((N_DENSE, D_FF, D_MODEL), D_FF, res_scale),
        "router_w": w((N_MOE, D_MODEL, N_EXPERTS), D_MODEL),
        "router_b": 0.01 * jax.random.normal(next(ks), (N_MOE, N_EXPERTS), f32),
        "exp_w_gate": w((N_MOE, N_EXPERTS, D_MODEL, D_FF), D_MODEL),
        "exp_w_up": w((N_MOE, N_EXPERTS, D_MODEL, D_FF), D_MODEL),
        "exp_w_down": w((N_MOE, N_EXPERTS, D_FF, D_MODEL), D_FF, res_scale),
    }


def reference(x, mem, norm_mix, w_in, sb_out_gain, swa_q_gain, swa_k_gain, swa_sinks,
              swa_out_gain, rel_bias, w_out, norm_xattn, norm_mem, xattn_wq, xattn_wkv,
              xattn_q_gain, xattn_k_gain, xattn_wo, norm_ffn, dense_w_gate, dense_w_up,
              dense_w_down, router_w, router_b, exp_w_gate, exp_w_up, exp_w_down):
    B, S, D = x.shape
    dist = WINDOW + np.arange(WINDOW)[:, None] - np.arange(2 * WINDOW)[None, :]
    swa_bias = jnp.transpose(rel_bias[t5_buckets(dist)], (2, 0, 1))
    o1 = 3 * SB_WIDTH
    o2 = o1 + SWA_WIDTH
    o3 = o2 + SWA_KV_WIDTH
    for l in range(DEPTH):
        h = rms_norm(x, norm_mix[l])
        proj = h @ w_in[l]
        sb_q = proj[..., 0:SB_WIDTH].reshape(B, S, SB_HEADS, HEAD_DIM).transpose(0, 2, 1, 3)
        sb_k = proj[..., SB_WIDTH:2 * SB_WIDTH].reshape(B, S, SB_HEADS, HEAD_DIM).transpose(0, 2, 1, 3)
        sb_v = proj[..., 2 * SB_WIDTH:o1].reshape(B, S, SB_HEADS, HEAD_DIM).transpose(0, 2, 1, 3)
        sb_o = stick_breaking_attention(sb_q, sb_k, sb_v).transpose(0, 2, 1, 3).reshape(B, S, SB_WIDTH)
        sw_q = rms_norm(proj[..., o1:o2].reshape(B, S, SWA_Q_HEADS, HEAD_DIM), swa_q_gain[l])
        sw_k = rms_norm(proj[..., o2:o3].reshape(B, S, SWA_KV_HEADS, HEAD_DIM), swa_k_gain[l])
        sw_v = proj[..., o3:].reshape(B, S, SWA_KV_HEADS, HEAD_DIM)
        sw_o = sliding_window_sink_attention(sw_q, sw_k, sw_v, swa_sinks[l], swa_bias)
        mixed = jnp.concatenate([rms_norm(sb_o, sb_out_gain[l]), rms_norm(sw_o, swa_out_gain[l])], axis=-1)
        x = x + mixed @ w_out[l]
        hx = rms_norm(x, norm_xattn[l])
        hm = rms_norm(mem, norm_mem[l])
        x = x + memory_cross_attention(hx, hm, xattn_wq[l], xattn_wkv[l],
                                       xattn_q_gain[l], xattn_k_gain[l]) @ xattn_wo[l]
        hf = rms_norm(x, norm_ffn[l])
        i = l // 2
        if l % 2 == 0:
            x = x + swiglu(hf, dense_w_gate[i], dense_w_up[i], dense_w_down[i])
        else:
            y = moe_swiglu(hf.reshape(B * S, D), router_w[i], router_b[i],
                           exp_w_gate[i], exp_w_up[i], exp_w_down[i])
            x = x + y.reshape(B, S, D)
    return x
```

```python
import contextlib
import numpy as np
import concourse.bass as bass
import concourse.mybir as mybir
from concourse.bass_utils import run_bass_kernel_spmd

F32 = mybir.dt.float32
BF16 = mybir.dt.bfloat16
AF = mybir.ActivationFunctionType
ALU = mybir.AluOpType
AX = mybir.AxisListType

S = 2048
D = 1024
NT = 16
DEPTH = 4
DFF = 3584
NE = 8
NM = 256
EPS = 1e-6
NEG = -30000.0


class Op:
    __slots__ = ("eng", "fn", "deps", "dma_key", "idx", "inc", "val")


class Rec:
    ENGS = ("pe", "act", "dve", "pool", "sp")

    def __init__(self):
        self.ops = {e: [] for e in self.ENGS}
        self.lastw = {}
        self.readers = {}
        self.dma_cnt = {}
        self.last_dma = {}
        self.floor = None
        self.nops = 0

    @staticmethod
    def _key(d):
        if d.dma_key is not None:
            return ("dma", d.dma_key)
        return ("eng", d.eng)

    def add(self, eng, fn, reads=(), writes=(), dma_key=None, extra=()):
        op = Op()
        op.eng = eng
        op.fn = fn
        op.dma_key = dma_key
        op.inc = dma_key is not None
        deps = {}

        def adddep(d):
            if d.dma_key is None and d.eng == "pe" and eng == "pe" and dma_key is None:
                return
            k = self._key(d)
            v = d.val if d.dma_key is not None else d.idx
            cur = deps.get(k)
            if cur is None or cur[0] < v:
                deps[k] = (v, d)

        if self.floor is not None:
            adddep(self.floor)
        for d in extra:
            adddep(d)
        for t in reads:
            d = self.lastw.get(t)
            if d is not None:
                adddep(d)
        for t in writes:
            d = self.lastw.get(t)
            if d is not None:
                adddep(d)
            rs = self.readers.get(t)
            if rs:
                for (_, r) in rs.values():
                    adddep(r)
        lst = self.ops[eng]
        op.idx = len(lst)
        if dma_key is not None:
            c = self.dma_cnt.get(dma_key, 0) + 16
            self.dma_cnt[dma_key] = c
            op.val = c
            self.last_dma[dma_key] = op
        else:
            op.val = None
        op.deps = [d for (_, d) in deps.values()]
        for d in op.deps:
            d.inc = True
        lst.append(op)
        self.nops += 1
        for t in writes:
            self.lastw[t] = op
            self.readers[t] = {}
        k = self._key(op)
        v = op.val if dma_key is not None else op.idx
        for t in reads:
            rs = self.readers.get(t)
            if rs is None:
                rs = self.readers[t] = {}
            rs[k] = (v, op)
        return op

    def barrier(self, fn):
        extra = []
        for e in ("pe", "act", "dve", "pool"):
            for op in reversed(self.ops[e]):
                if op.dma_key is None and op.fn is not None:
                    extra.append(op)
                    break
        extra.extend(self.last_dma.values())
        b = self.add("pool", fn, (), (), extra=extra)
        self.floor = b
        return b

    def dma(self, eng, out, in_, reads, writes, key, **kw):
        return self.add(eng, lambda e: e.dma_start(out=out, in_=in_, **kw), reads, writes, dma_key=key)

    def emit(self, nc):
        for e in self.ENGS:
            c = 0
            for op in self.ops[e]:
                if op.dma_key is None:
                    if op.inc:
                        c += 1
                    op.val = c
        with contextlib.ExitStack() as st:
            sems = {}
            for e in ("pe", "act", "dve", "pool"):
                sems[("eng", e)] = st.enter_context(nc.semaphore("s_" + e))
            for i, k in enumerate(self.dma_cnt.keys()):
                sems[("dma", k)] = st.enter_context(nc.semaphore("d%d" % i))
            block = st.enter_context(nc.Block())

            def replay(ename):
                def body(eng):
                    waited = {}
                    for op in self.ops[ename]:
                        for d in op.deps:
                            k = self._key(d)
                            if waited.get(k, 0) < d.val:
                                eng.wait_ge(sems[k], d.val)
                                waited[k] = d.val
                        if op.fn is None:
                            continue
                        ins = op.fn(eng)
                        if op.dma_key is not None:
                            ins.then_inc(sems[("dma", op.dma_key)], 16)
                        elif op.inc:
                            ins.then_inc(sems[("eng", ename)], 1)
                return body

            block.tensor(replay("pe"))
            block.scalar(replay("act"))
            block.vector(replay("dve"))
            block.gpsimd(replay("pool"))
            block.sync(replay("sp"))
        return len(self.dma_cnt)


W_NAMES = ["norm_mix", "w_in", "sb_out_gain", "swa_q_gain", "swa_k_gain", "swa_sinks", "swa_out_gain",
           "w_out", "norm_xattn", "norm_mem", "xattn_wq", "xattn_wkv", "xattn_q_gain", "xattn_k_gain",
           "xattn_wo", "norm_ffn", "dense_w_gate", "dense_w_up", "dense_w_down", "router_w", "router_b",
           "exp_w_gate", "exp_w_up", "exp_w_down"]
W_SHAPES = {
    "norm_mix": [DEPTH, D], "w_in": [DEPTH, D, 2304], "sb_out_gain": [DEPTH, 512], "swa_q_gain": [DEPTH, 64],
    "swa_k_gain": [DEPTH, 64], "swa_sinks": [DEPTH, 8], "swa_out_gain": [DEPTH, 512], "w_out": [DEPTH, D, D],
    "norm_xattn": [DEPTH, D], "norm_mem": [DEPTH, D], "xattn_wq": [DEPTH, D, 512], "xattn_wkv": [DEPTH, D, 1024],
    "xattn_q_gain": [DEPTH, 128], "xattn_k_gain": [DEPTH, 128], "xattn_wo": [DEPTH, 512, D], "norm_ffn": [DEPTH, D],
    "dense_w_gate": [2, D, DFF], "dense_w_up": [2, D, DFF], "dense_w_down": [2, DFF, D], "router_w": [2, D, NE],
    "router_b": [2, NE], "exp_w_gate": [2, NE, D, DFF], "exp_w_up": [2, NE, D, DFF], "exp_w_down": [2, NE, DFF, D],
}


def build_program(nseq=4, layers=(0, 1, 2, 3), phases=("mix", "xat", "ffn")):
    nc = bass.Bass("TRN2", target_bir_lowering=False)
    x_d = nc.dram_tensor("x", [nseq, S, D], F32, kind="ExternalInput").ap()
    mem_d = nc.dram_tensor("mem", [nseq, NM, D], F32, kind="ExternalInput").ap()
    biast_d = nc.dram_tensor("swa_bias_t", [256, 8, 128], F32, kind="ExternalInput").ap()
    wd_ = {n: nc.dram_tensor(n, W_SHAPES[n], F32, kind="ExternalInput").ap() for n in W_NAMES}
    y_d = nc.dram_tensor("y", [nseq, S, D], F32, kind="ExternalOutput").ap()
    mixed_d = nc.dram_tensor("mixed_scr", [S, D], BF16).ap()
    R = Rec()

    with contextlib.ExitStack() as st:
        x = st.enter_context(nc.sbuf_tensor("sb_x", [128, NT, D], F32))
        hT = st.enter_context(nc.sbuf_tensor("sb_hT", [128, 8, S], BF16))
        wpool = st.enter_context(nc.sbuf_tensor("sb_w", [128, 6, 4096], BF16))
        arena = st.enter_context(nc.sbuf_tensor("sb_arena", [128, 14336], F32))
        cst = st.enter_context(nc.sbuf_tensor("sb_cst", [128, 320], F32))
        rw = st.enter_context(nc.sbuf_tensor("sb_rw", [128, 2, 8, NE], F32))
        identf = st.enter_context(nc.sbuf_tensor("sb_identf", [128, 128], F32))
        identb = st.enter_context(nc.sbuf_tensor("sb_identb", [128, 128], BF16))
        negtri = st.enter_context(nc.sbuf_tensor("sb_negtri", [128, 128], BF16))
        onesb = st.enter_context(nc.sbuf_tensor("sb_ones", [128, 128], BF16))
        maskd = st.enter_context(nc.sbuf_tensor("sb_maskd", [128, 128], BF16))
        tmpf = st.enter_context(nc.sbuf_tensor("sb_tmpf", [128, 128], F32))
        dummy = st.enter_context(nc.sbuf_tensor("sb_dummy", [128, 2], F32))
        stat = st.enter_context(nc.sbuf_tensor("sb_stat", [128, 64], F32))
        ps = st.enter_context(nc.psum_tensor("ps", [128, 8, 512], F32))

        def av(off, nbytes):
            assert off % 4 == 0 and nbytes % 4 == 0 and off + nbytes <= 57344, (off, nbytes)
            return arena[:, off // 4:(off + nbytes) // 4]

        def avb(off, nelem):
            return av(off, nelem * 2).bitcast(BF16)

        KB = 1024
        hn = [avb(0, 1024), avb(2 * KB, 1024)]
        junk = avb(4 * KB, 1024)

        def ccol(l, k):
            return l * 48 + k
        C_GMIX, C_GXAT, C_GFFN, C_GOUT, C_GMEM, C_SWQ, C_SWK, C_XQ, C_XK = 0, 8, 16, 24, 32, 40, 41, 42, 43
        C_ESINK = 192
        C_RB = 224

        bar_n = [0]

        def barrier():
            bar_n[0] += 1
            R.barrier(lambda e: e.memset(dummy[:, 0:1], 0.0))

        def cdma(out, in_, tok, **kw):
            R.dma("sp", out, in_, [], [tok], key=("c", tok), **kw)

        cidx = [0]

        def cload(out, in_, **kw):
            cidx[0] += 1
            R.dma("sp", out, in_, [], [("cst", cidx[0])], key=("c", cidx[0] % 4), **kw)

        def gT_load(col, vec):
            n = vec.shape[0] // 128
            cload(cst[:, col:col + n], vec.rearrange("(c p) -> p c", p=128), allow_slow_non_contiguous=True)

        for l in range(DEPTH):
            gT_load(ccol(l, C_GMIX), wd_["norm_mix"][l])
            gT_load(ccol(l, C_GXAT), wd_["norm_xattn"][l])
            gT_load(ccol(l, C_GFFN), wd_["norm_ffn"][l])
            gT_load(ccol(l, C_GOUT), wd_["sb_out_gain"][l])
            gT_load(ccol(l, C_GOUT) + 4, wd_["swa_out_gain"][l])
            gT_load(ccol(l, C_GMEM), wd_["norm_mem"][l])
            gT_load(ccol(l, C_XQ), wd_["xattn_q_gain"][l])
            gT_load(ccol(l, C_XK), wd_["xattn_k_gain"][l])
            for half in range(2):
                cload(cst[half * 64:(half + 1) * 64, ccol(l, C_SWQ):ccol(l, C_SWQ) + 1],
                      wd_["swa_q_gain"][l].rearrange("(d o) -> d o", o=1), allow_slow_non_contiguous=True)
                cload(cst[half * 64:(half + 1) * 64, ccol(l, C_SWK):ccol(l, C_SWK) + 1],
                      wd_["swa_k_gain"][l].rearrange("(d o) -> d o", o=1), allow_slow_non_contiguous=True)
        cload(cst[:, C_ESINK:C_ESINK + 32], wd_["swa_sinks"].rearrange("l h -> (l h)").partition_broadcast(128))
        cload(cst[:, C_RB:C_RB + 16], wd_["router_b"].rearrange("i e -> (i e)").partition_broadcast(128))
        for i in range(2):
            cload(rw[:, i, :, :], wd_["router_w"][i].rearrange("(c p) e -> p c e", p=128),
                  allow_slow_non_contiguous=True)
        R.add("pool", lambda e: e.memset(tmpf[:, :], 1.0), [], ["tmpf"])
        R.add("pool", lambda e: e.affine_select(out=identf[:, :], in_=tmpf[:, :], pattern=[[-1, 128]],
                                                compare_op=ALU.is_equal, fill=0.0, base=0, channel_multiplier=1),
              ["tmpf"], ["identf"])
        R.add("pool", lambda e: e.tensor_copy(out=identb[:, :], in_=identf[:, :]), ["identf"], ["identb"])
        R.add("pool", lambda e: e.tensor_copy(out=onesb[:, :], in_=tmpf[:, :]), ["tmpf"], ["onesb"])
        R.add("pool", lambda e: e.affine_select(out=maskd[:, :], in_=tmpf[:, :], pattern=[[1, 128]],
                                                compare_op=ALU.is_gt, fill=0.0, base=0, channel_multiplier=-1),
              ["tmpf"], ["maskd"])
        R.add("pool", lambda e: e.memset(tmpf[:, :], -1.0), ["tmpf"], ["tmpf"])
        R.add("pool", lambda e: e.affine_select(out=negtri[:, :], in_=tmpf[:, :], pattern=[[-1, 128]],
                                                compare_op=ALU.is_ge, fill=0.0, base=0, channel_multiplier=1),
              ["tmpf"], ["negtri"])
        barrier()
        R.add("act", lambda e: e.activation(out=cst[:, C_ESINK:C_ESINK + 32], in_=cst[:, C_ESINK:C_ESINK + 32],
                                            func=AF.Exp), [], ["esink"])
        barrier()

        wctr = [0]

        def wslot():
            k = wctr[0] % 6
            wctr[0] += 1
            return k

        def wload(k, part, col0, ncols, src):
            raise NotImplementedError

        def norm_T(src_fn, src_tok_fn, ntiles, groups, gcol, dst_fn, dst_tok_fn, extra_tile_fn=None):
            ng = len(groups)
            W = groups[-1][1]
            nch = W // 128
            for t in range(ntiles):
                b = t % 2
                src = src_fn(t)
                stok = src_tok_fn(t)
                ssv = stat[:, 0:ng]
                for gi, (c0, c1) in enumerate(groups):
                    R.add("act", lambda e, src=src, c0=c0, c1=c1, gi=gi: e.activation(
                        out=junk[:, c0:c1], in_=src[:, c0:c1], func=AF.Square, accum_out=stat[:, gi:gi + 1]),
                        [stok], ["junk", "stat"])
                wdt = groups[0][1] - groups[0][0]
                R.add("act", lambda e, ssv=ssv: e.activation(out=stat[:, 8:8 + ng], in_=ssv, func=AF.Ln, bias=EPS,
                                                            scale=1.0 / wdt), ["stat"], ["stat2"])
                R.add("act", lambda e, b=b: e.activation(out=stat[:, 16 + b * 4:16 + b * 4 + ng],
                                                         in_=stat[:, 8:8 + ng], func=AF.Exp, scale=-0.5),
                      ["stat2"], [("rstd", b)])
                for gi, (c0, c1) in enumerate(groups):
                    R.add("dve", lambda e, src=src, c0=c0, c1=c1, gi=gi, b=b: e.tensor_scalar(
                        out=hn[b][:, c0:c1], in0=src[:, c0:c1], scalar1=stat[:, 16 + b * 4 + gi:17 + b * 4 + gi],
                        scalar2=None, op0=ALU.mult), [stok, ("rstd", b)], [("hn", b)])
                if extra_tile_fn is not None:
                    extra_tile_fn(t, b, src, stok)
                tp = ps[:, b, :].bitcast(BF16)
                for c in range(nch):
                    R.add("pe", lambda e, c=c, b=b, tp=tp: e.transpose(
                        out=tp[:, c * 128:(c + 1) * 128], in_=hn[b][:, c * 128:(c + 1) * 128], identity=identb[:, :]),
                        [("hn", b), "identb"], [("ps", b)])
                dst = dst_fn(t)
                R.add("dve", lambda e, tp=tp, dst=dst: e.tensor_tensor(
                    out=dst, in0=tp[:, 0:nch * 128].rearrange("p (c n) -> p c n", c=nch),
                    in1=cst[:, gcol:gcol + nch].unsqueeze(2).to_broadcast([128, nch, 128]), op=ALU.mult),
                    [("ps", b)], [dst_tok_fn(t)])

        def x_norm_T(gcol, extra_tile_fn=None):
            norm_T(lambda t: x[:, t, :], lambda t: ("x", t), NT, [(0, D)], gcol,
                   lambda t: hT[:, :, t * 128:(t + 1) * 128], lambda t: ("hT", t), extra_tile_fn)

        def add_to_x(t, pb, gate_ap=None):
            src = ps[:, pb:pb + 2, :].rearrange("p a n -> p (a n)")
            if gate_ap is None:
                R.add("dve", lambda e: e.tensor_tensor(out=x[:, t, :], in0=src, in1=x[:, t, :], op=ALU.add),
                      [("ps", pb), ("ps", pb + 1), ("x", t)], [("x", t)])
            else:
                R.add("dve", lambda e: e.scalar_tensor_tensor(out=x[:, t, :], in0=src, scalar=gate_ap, in1=x[:, t, :],
                                                              op0=ALU.mult, op1=ALU.add),
                      [("ps", pb), ("ps", pb + 1), ("x", t), "gates"], [("x", t)])

        def out_proj(wsrc, nch):
            ks = []
            for half in range(2):
                k = wslot()
                wv = wpool[:, k, 0:nch * 512].rearrange("p (c n) -> p c n", c=nch)
                R.dma("pool", wv, wsrc[:, half * 512:(half + 1) * 512].rearrange("(c p) n -> p c n", p=128),
                      [], [("w", k, 0)], key=("w", k, 0))
                ks.append((k, wv))
            for t in range(NT):
                pb = 4 + (t % 2) * 2
                for half in range(2):
                    k, wv = ks[half]
                    for c in range(nch):
                        R.add("pe", lambda e, c=c, half=half, wv=wv, pb=pb, t=t: e.matmul(
                            ps[:, pb + half, :], lhsT=hT[:, c, t * 128:(t + 1) * 128], rhs=wv[:, c, :],
                            start=(c == 0), stop=(c == nch - 1)), [("hT", t), ("w", k, 0)], [("ps", pb + half)])
                add_to_x(t, pb)

        def mixer(l):
            w_in = wd_["w_in"][l]
            x_norm_T(ccol(l, C_GMIX))
            PB = 6 * KB

            def pair_bufs(i):
                o = PB + i * 12 * KB
                return (avb(o, 2048), avb(o + 4 * KB, 2048),
                        avb(o + 8 * KB, 2048).rearrange("p (t d) -> p t d", t=16))
            SO = PB + 24 * KB
            ebuf = [av(SO + i * 2 * KB, 2 * KB) for i in range(2)]
            spb = [avb(SO + 4 * KB + i * KB, 512) for i in range(2)]
            lgA = [av(SO + 6 * KB + i * 2 * KB, 2 * KB) for i in range(2)]
            Ab = [avb(SO + 10 * KB + i * KB, 512) for i in range(2)]
            Rb = [av(SO + 12 * KB + i * 2 * KB, 2 * KB) for i in range(2)]
            ost = [avb(SO + 16 * KB + i * KB, 512).rearrange("p (t d) -> p t d", t=4) for i in range(2)]

            def sb_proj(p):
                pi = p % 2
                qT, kT, vv = pair_bufs(pi)
                k = wslot()
                wv = wpool[:, k, 0:8 * 384].rearrange("p (c n) -> p c n", c=8)
                for part in range(3):
                    c0 = part * 512 + p * 128
                    R.dma("pool", wv[:, :, part * 128:(part + 1) * 128],
                          w_in[:, c0:c0 + 128].rearrange("(c p) n -> p c n", p=128), [], [("w", k, part)],
                          key=("w", k, part))
                for part, dst in ((0, qT), (1, kT)):
                    for tg in range(4):
                        for c in range(8):
                            R.add("pe", lambda e, c=c, tg=tg, part=part, wv=wv: e.matmul(
                                ps[:, 7, :], lhsT=wv[:, c, part * 128:(part + 1) * 128],
                                rhs=hT[:, c, tg * 512:(tg + 1) * 512], start=(c == 0), stop=(c == 7)),
                                [("hT", tg * 4 + i) for i in range(4)] + [("w", k, part)], [("ps", 7)])
                        sc = 0.125 if part == 0 else 1.0
                        R.add("dve", lambda e, dst=dst, tg=tg, sc=sc: e.tensor_scalar(
                            out=dst[:, tg * 512:(tg + 1) * 512], in0=ps[:, 7, :], scalar1=sc, scalar2=None,
                            op0=ALU.mult), [("ps", 7)], [("pq", pi, part, tg)])
                for t4 in range(4):
                    for tt in range(4):
                        t = t4 * 4 + tt
                        for c in range(8):
                            R.add("pe", lambda e, c=c, t=t, tt=tt, wv=wv: e.matmul(
                                ps[:, 7, tt * 128:(tt + 1) * 128], lhsT=hT[:, c, t * 128:(t + 1) * 128],
                                rhs=wv[:, c, 256:384], start=(c == 0), stop=(c == 7)),
                                [("hT", t), ("w", k, 2)], [("ps", 7)])
                    R.add("dve", lambda e, vv=vv, t4=t4: e.tensor_copy(
                        out=vv[:, t4 * 4:(t4 + 1) * 4, :], in_=ps[:, 7, :].rearrange("p (t d) -> p t d", t=4)),
                        [("ps", 7)], [("pv", pi, t4)])

            def sb_attn(p):
                pi = p % 2
                qT, kT, vv = pair_bufs(pi)
                items = []
                for g in range(4):
                    for hp in range(2):
                        for a in range(4 * g + 3, -1, -1):
                            items.append((g, hp, a))
                ucount = [0]

                def unit_of(g, hp):
                    return g * 2 + hp

                def cols(g, a):
                    j = max(a - 4 * g, 0)
                    return j * 128, 512 - j * 128

                def st1(it, k):
                    g, hp, a = it
                    c0, n = cols(g, a)
                    pb = hp * 64
                    zb = k % 2
                    R.add("pe", lambda e: e.matmul(ps[:, zb, 0:n], lhsT=kT[pb:pb + 64, a * 128:(a + 1) * 128],
                                                   rhs=qT[pb:pb + 64, g * 512 + c0:(g + 1) * 512], start=True,
                                                   stop=True),
                          [("pq", pi, 0, g), ("pq", pi, 1, a // 4)], [("ps", zb)])
                    R.add("act", lambda e: e.activation(out=ebuf[zb][:, 0:n], in_=ps[:, zb, 0:n], func=AF.Exp),
                          [("ps", zb)], [("e", zb)])

                def st1b(it, k):
                    g, hp, a = it
                    c0, n = cols(g, a)
                    zb = k % 2
                    R.add("act", lambda e: e.activation(out=spb[zb][:, 0:n], in_=ebuf[zb][:, 0:n], func=AF.Ln,
                                                        bias=1.0), [("e", zb)], [("sp", zb)])
                    if a >= 4 * g:
                        R.add("pool", lambda e: e.tensor_tensor(out=spb[zb][:, 0:128], in0=spb[zb][:, 0:128],
                                                                in1=maskd[:, :], op=ALU.mult),
                              [("sp", zb), "maskd"], [("sp", zb)])

                def st2a(it, k):
                    g, hp, a = it
                    c0, n = cols(g, a)
                    pb = hp * 64
                    zb = k % 2
                    u = unit_of(g, hp) % 2
                    if a == 4 * g + 3:
                        R.add("pool", lambda e: e.memset(Rb[u][:, :], 0.0), [], [("R", u)])
                        R.add("dve", lambda e: e.memset(ps[:, 5 + u, 0:256], 0.0), [], [("ps", 5 + u)])
                    R.add("pe", lambda e: e.matmul(ps[:, 2 + zb, 0:n], lhsT=kT[pb:pb + 64, a * 128:(a + 1) * 128],
                                                   rhs=qT[pb:pb + 64, g * 512 + c0:(g + 1) * 512], start=True,
                                                   stop=False),
                          [("pq", pi, 0, g), ("pq", pi, 1, a // 4)], [("ps", 2 + zb)])
                    R.add("pe", lambda e: e.matmul(ps[:, 2 + zb, 0:n], lhsT=negtri[:, :], rhs=spb[zb][:, 0:n],
                                                   start=False, stop=True), [("sp", zb), "negtri"], [("ps", 2 + zb)])
                    R.add("pe", lambda e: e.matmul(ps[:, 4, 0:n], lhsT=onesb[:, :], rhs=spb[zb][:, 0:n], start=True,
                                                   stop=True), [("sp", zb), "onesb"], [("ps", 4)])
                    R.add("dve", lambda e: e.tensor_tensor(out=lgA[zb][:, 0:n], in0=ps[:, 2 + zb, 0:n],
                                                           in1=Rb[u][:, c0:512], op=ALU.subtract),
                          [("ps", 2 + zb), ("R", u)], [("lgA", zb)])
                    R.add("dve", lambda e: e.tensor_tensor(out=Rb[u][:, c0:512], in0=ps[:, 4, 0:n],
                                                           in1=Rb[u][:, c0:512], op=ALU.add),
                          [("ps", 4), ("R", u)], [("R", u)])

                def st2b(it, k):
                    g, hp, a = it
                    c0, n = cols(g, a)
                    zb = k % 2
                    R.add("act", lambda e: e.activation(out=Ab[zb][:, 0:n], in_=lgA[zb][:, 0:n], func=AF.Exp),
                          [("lgA", zb)], [("A", zb)])
                    if a >= 4 * g:
                        R.add("pool", lambda e: e.tensor_tensor(out=Ab[zb][:, 0:128], in0=Ab[zb][:, 0:128],
                                                                in1=maskd[:, :], op=ALU.mult),
                              [("A", zb), "maskd"], [("A", zb)])

                def st3(it, k):
                    g, hp, a = it
                    c0, n = cols(g, a)
                    zb = k % 2
                    u = unit_of(g, hp) % 2
                    ob = 5 + u
                    j0 = c0 // 128
                    for jj in range(j0, 4):
                        tile = 4 * g + jj
                        R.add("pe", lambda e, jj=jj, tile=tile: e.matmul(
                            ps[:, ob, jj * 64:(jj + 1) * 64], lhsT=Ab[zb][:, (jj - j0) * 128:(jj - j0 + 1) * 128],
                            rhs=vv[:, a, hp * 64:(hp + 1) * 64], start=False, stop=(a == 0),
                            skip_group_check=True),
                            [("A", zb), ("pv", pi, a // 4)], [("ps", ob)])
                    if a == 0:
                        osb = g % 2
                        R.add("dve", lambda e: e.tensor_copy(
                            out=ost[osb][:, :, hp * 64:(hp + 1) * 64],
                            in_=ps[:, ob, 0:256].rearrange("p (t d) -> p t d", t=4)),
                            [("ps", ob)], [("ost", osb)])
                        if hp == 1:
                            R.dma("sp", mixed_d[g * 512:(g + 1) * 512, p * 128:(p + 1) * 128].rearrange(
                                "(t q) d -> q t d", q=128), ost[osb][:, :, :], [("ost", osb)],
                                [("mixed", g * 4 + i) for i in range(4)], key=("ost", osb))

                n_it = len(items)
                for k in range(n_it + 2):
                    if k < n_it:
                        st1(items[k], k)
                    if 1 <= k <= n_it:
                        st2b(items[k - 1], k - 1)
                    if k < n_it:
                        st1b(items[k], k)
                        st2a(items[k], k)
                    if 1 <= k <= n_it:
                        st3(items[k - 1], k - 1)

            if "sb" in phases or "mix" in phases:
                sb_proj(0)
                for p in range(4):
                    if p + 1 < 4:
                        sb_proj(p + 1)
                    sb_attn(p)
            barrier()
            A0 = 6 * KB
            qTs = avb(A0, 8192).rearrange("p (i n) -> p i n", i=4)
            kTs = avb(A0 + 16 * KB, 2048)
            vsw = avb(A0 + 20 * KB, 16 * 2 * 65 + 32)[:, 0:16 * 2 * 65].rearrange("p (t g d) -> p t g d", t=16, g=2)
            bia = av(A0 + 25 * KB, 8 * KB).rearrange("p (c h q) -> p c h q", c=2, h=8)
            sq = av(A0 + 33 * KB, 640 * 4)
            qn = [avb(A0 + 36 * KB + i * 1536, 640) for i in range(2)]
            sbf = [av(A0 + 39 * KB + i * 2 * KB, 2 * KB) for i in range(2)]
            Pb = [avb(A0 + 43 * KB + i * KB, 512) for i in range(4)]
            osw = [avb(A0 + 47 * KB + i * 512, 256) for i in range(2)]
            sml = av(A0 + 48 * KB, 256)

            if "swa" in phases or "mix" in phases:
                kq = wslot()
                wq_v = wpool[:, kq, :].rearrange("p (c n) -> p c n", c=8)
                R.dma("pool", wq_v, w_in[:, 1536:2048].rearrange("(c p) n -> p c n", p=128), [], [("w", kq, 0)],
                      key=("w", kq, 0))
                kk = wslot()
                wkv_v = wpool[:, kk, 0:2048].rearrange("p (c n) -> p c n", c=8)
                R.dma("pool", wkv_v, w_in[:, 2048:2304].rearrange("(c p) n -> p c n", p=128), [], [("w", kk, 0)],
                      key=("w", kk, 0))
                R.dma("sp", bia, biast_d.rearrange("(c p) h q -> p c h q", p=128), [], ["bia"], key="bia")
                R.add("pool", lambda e: e.affine_select(out=bia[:, 0, :, :], in_=bia[:, 0, :, :],
                                                        pattern=[[0, 8], [-1, 128]], compare_op=ALU.is_gt, fill=NEG,
                                                        base=0, channel_multiplier=1), ["bia"], ["bia"])
                R.add("pool", lambda e: e.affine_select(out=bia[:, 1, :, :], in_=bia[:, 1, :, :],
                                                        pattern=[[0, 8], [1, 128]], compare_op=ALU.is_ge, fill=NEG,
                                                        base=0, channel_multiplier=-1), ["bia"], ["bia"])
                R.add("pool", lambda e: e.memset(vsw[:, :, :, 64:65], 1.0), [], ["vsw1"])
                for t in range(NT):
                    b = t % 2
                    pb = 4 + b * 2
                    for c in range(8):
                        R.add("pe", lambda e, c=c, t=t, pb=pb: e.matmul(
                            ps[:, pb, :], lhsT=hT[:, c, t * 128:(t + 1) * 128], rhs=wq_v[:, c, :], start=(c == 0),
                            stop=(c == 7)), [("hT", t), ("w", kq, 0)], [("ps", pb)])
                    for c in range(8):
                        R.add("pe", lambda e, c=c, t=t, pb=pb: e.matmul(
                            ps[:, pb + 1, 0:256], lhsT=hT[:, c, t * 128:(t + 1) * 128], rhs=wkv_v[:, c, :],
                            start=(c == 0), stop=(c == 7)), [("hT", t), ("w", kk, 0)], [("ps", pb + 1)])
                    qk = ps[:, pb:pb + 2, :].rearrange("p a n -> p (a n)")[:, 0:640]
                    R.add("act", lambda e, qk=qk: e.activation(out=sq[:, :], in_=qk, func=AF.Square),
                          [("ps", pb), ("ps", pb + 1)], ["sq"])
                    R.add("dve", lambda e: e.tensor_reduce(out=sml[:, 0:10], in_=sq.rearrange("p (h d) -> p h d", d=64),
                                                           axis=AX.X, op=ALU.add), ["sq"], ["sml"])
                    R.add("act", lambda e: e.activation(out=sml[:, 16:26], in_=sml[:, 0:10], func=AF.Ln, bias=EPS,
                                                        scale=1.0 / 64), ["sml"], ["sml2"])
                    R.add("act", lambda e, b=b: e.activation(out=sml[:, 32 + b * 16:42 + b * 16], in_=sml[:, 16:26],
                                                             func=AF.Exp, scale=-0.5), ["sml2"], [("srs", b)])
                    R.add("dve", lambda e, b=b, qk=qk: e.tensor_tensor(
                        out=qn[b].rearrange("p (h d) -> p h d", d=64), in0=qk.rearrange("p (h d) -> p h d", d=64),
                        in1=sml[:, 32 + b * 16:42 + b * 16].unsqueeze(2).to_broadcast([128, 10, 64]), op=ALU.mult),
                        [("ps", pb), ("ps", pb + 1), ("srs", b)], [("qn", b)])
                    R.add("dve", lambda e, t=t, pb=pb: e.tensor_copy(
                        out=vsw[:, t, :, 0:64], in_=ps[:, pb + 1, 128:256].rearrange("p (g d) -> p g d", g=2)),
                        [("ps", pb + 1)], [("vsw", t)])
                    tp = ps[:, b, :].bitcast(BF16)
                    for c in range(5):
                        R.add("pe", lambda e, c=c, b=b, tp=tp: e.transpose(
                            out=tp[:, c * 128:(c + 1) * 128], in_=qn[b][:, c * 128:(c + 1) * 128],
                            identity=identb[:, :]), [("qn", b), "identb"], [("ps", b)])
                    R.add("dve", lambda e, t=t, tp=tp: e.tensor_scalar(
                        out=qTs[:, :, t * 128:(t + 1) * 128], in0=tp[:, 0:512].rearrange("p (i n) -> p i n", i=4),
                        scalar1=cst[:, ccol(l, C_SWQ):ccol(l, C_SWQ) + 1], scalar2=0.125, op0=ALU.mult,
                        op1=ALU.mult), [("ps", b)], [("qTs", t)])
                    R.add("dve", lambda e, t=t, tp=tp: e.tensor_scalar(
                        out=kTs[:, t * 128:(t + 1) * 128], in0=tp[:, 512:640],
                        scalar1=cst[:, ccol(l, C_SWK):ccol(l, C_SWK) + 1], scalar2=None, op0=ALU.mult),
                        [("ps", b)], [("kTs", t)])
                it = 0
                for n in range(NT):
                    for g in range(2):
                        ob = 6 + ((n * 2 + g) % 2)
                        chunks = [(1, n)] if n == 0 else [(0, n - 1), (1, n)]
                        slots = []
                        for ci, (cc, kt) in enumerate(chunks):
                            sbk = it % 4
                            it += 1
                            slots.append(sbk)
                            zb = 2 + (sbk % 2)
                            R.add("pe", lambda e, kt=kt, n=n, g=g, zb=zb: e.matmul(
                                ps[:, zb, :], lhsT=kTs[g * 64:(g + 1) * 64, kt * 128:(kt + 1) * 128],
                                rhs=qTs[g * 64:(g + 1) * 64, :, n * 128:(n + 1) * 128], start=True, stop=True),
                                [("kTs", kt), ("qTs", n)], [("ps", zb)])
                            R.add("dve", lambda e, zb=zb, sbk=sbk, cc=cc, g=g: e.tensor_tensor(
                                out=sbf[sbk % 2].rearrange("p (i q) -> p i q", i=4),
                                in0=ps[:, zb, :].rearrange("p (i q) -> p i q", i=4),
                                in1=bia[:, cc, g * 4:(g + 1) * 4, :], op=ALU.add), [("ps", zb), "bia"],
                                [("sbf", sbk % 2)])
                            R.add("act", lambda e, sbk=sbk: e.activation(out=Pb[sbk][:, :], in_=sbf[sbk % 2][:, :],
                                                                         func=AF.Exp), [("sbf", sbk % 2)], [("P", sbk)])
                        for i in range(4):
                            for ci, (cc, kt) in enumerate(chunks):
                                sbk = slots[ci]
                                R.add("pe", lambda e, i=i, sbk=sbk, kt=kt, g=g, ob=ob, ci=ci, nck=len(chunks): e.matmul(
                                    ps[:, ob, i * 65:(i + 1) * 65], lhsT=Pb[sbk][:, i * 128:(i + 1) * 128],
                                    rhs=vsw[:, kt, g, :], start=(ci == 0), stop=(ci == nck - 1)),
                                    [("P", sbk), ("vsw", kt), "vsw1"], [("ps", ob)])
                        ov = ps[:, ob, 0:260].rearrange("p (i d) -> p i d", i=4)
                        osb = (n * 2 + g) % 2
                        R.add("dve", lambda e, ov=ov, g=g: e.tensor_tensor(
                            out=sml[:, 0:4], in0=ov[:, :, 64],
                            in1=cst[:, C_ESINK + l * 8 + g * 4:C_ESINK + l * 8 + g * 4 + 4], op=ALU.add),
                            [("ps", ob), "esink"], ["den"])
                        R.add("dve", lambda e: e.reciprocal(out=sml[:, 4:8], in_=sml[:, 0:4]), ["den"], ["rec"])
                        R.add("dve", lambda e, ov=ov, osb=osb: e.tensor_tensor(
                            out=osw[osb].rearrange("p (i d) -> p i d", i=4), in0=ov[:, :, 0:64],
                            in1=sml[:, 4:8].unsqueeze(2).to_broadcast([128, 4, 64]), op=ALU.mult),
                            [("ps", ob), "rec"], [("osw", osb)])
                        R.dma("sp", mixed_d[n * 128:(n + 1) * 128, 512 + g * 256:512 + (g + 1) * 256], osw[osb][:, :],
                              [("osw", osb)], [("mixed", n)], key=("osw", osb))
            barrier()
            raw = [avb(6 * KB + i * 2 * KB, 1024) for i in range(2)]

            def raw_src(t):
                b = t % 2
                R.dma("sp", raw[b][:, :], mixed_d[t * 128:(t + 1) * 128, :], [("mixed", t)], [("raw", b)],
                      key=("raw", b))
                return raw[b]
            norm_T(raw_src, lambda t: ("raw", t % 2), NT, [(0, 512), (512, 1024)], ccol(l, C_GOUT),
                   lambda t: hT[:, :, t * 128:(t + 1) * 128], lambda t: ("hT", t))
            out_proj(wd_["w_out"][l], 8)
            barrier()

        def xattn(l, s):
            A0 = 6 * KB
            memt = [av(A0 + i * 4 * KB, 4 * KB) for i in range(2)]
            hmT = avb(A0 + 8 * KB, 2048).rearrange("p (c n) -> p c n", c=8)
            kTx = avb(A0 + 12 * KB, 1024).rearrange("p (h n) -> p h n", h=4)
            vx = avb(A0 + 14 * KB, 2 * 4 * 129 + 16)[:, 0:2 * 4 * 129].rearrange("p (m h d) -> p m h d", m=2, h=4)
            qTx = avb(A0 + 17 * KB, 8192).rearrange("p (h n) -> p h n", h=4)
            sq = av(A0 + 33 * KB, 2 * KB)
            qn = [avb(A0 + 35 * KB + i * KB, 512) for i in range(2)]
            Pb = [avb(A0 + 37 * KB + i * KB, 512) for i in range(4)]
            xor_ = [avb(A0 + 41 * KB + i * 4 * KB, 2048).rearrange("p (j n) -> p j n", j=4) for i in range(2)]
            sml = av(A0 + 49 * KB, 256)
            kwq = wslot()
            wq_v = wpool[:, kwq, :].rearrange("p (c n) -> p c n", c=8)
            R.dma("pool", wq_v, wd_["xattn_wq"][l].rearrange("(c p) n -> p c n", p=128), [], [("w", kwq, 0)],
                  key=("w", kwq, 0))
            kvs = []
            for half in range(2):
                k = wslot()
                wv = wpool[:, k, :].rearrange("p (c n) -> p c n", c=8)
                R.dma("pool", wv, wd_["xattn_wkv"][l][:, half * 512:(half + 1) * 512].rearrange(
                    "(c p) n -> p c n", p=128), [], [("w", k, 0)], key=("w", k, 0))
                kvs.append((k, wv))
            def mem_src(t):
                R.dma("sp", memt[t][:, :], mem_d[s, t * 128:(t + 1) * 128, :], [], [("memt", t)], key=("memt", t))
                return memt[t]
            norm_T(mem_src, lambda t: ("memt", t), 2, [(0, D)], ccol(l, C_GMEM),
                   lambda t: hmT[:, :, t * 128:(t + 1) * 128], lambda t: ("hmT", t))
            x_norm_T(ccol(l, C_GXAT))
            R.add("pool", lambda e: e.memset(vx[:, :, :, 128:129], 1.0), [], ["vx1"])

            def head_norm_T(pb, srcv, b, nh, gcolumn, scale, dst, dtok, stoks):
                R.add("act", lambda e: e.activation(out=sq[:, 0:nh * 128], in_=srcv, func=AF.Square), stoks, ["sq"])
                R.add("dve", lambda e: e.tensor_reduce(out=sml[:, 0:nh],
                                                       in_=sq[:, 0:nh * 128].rearrange("p (h d) -> p h d", d=128),
                                                       axis=AX.X, op=ALU.add), ["sq"], ["sml"])
                R.add("act", lambda e: e.activation(out=sml[:, 16:16 + nh], in_=sml[:, 0:nh], func=AF.Ln, bias=EPS,
                                                    scale=1.0 / 128), ["sml"], ["sml2"])
                R.add("act", lambda e: e.activation(out=sml[:, 32 + b * 8:32 + b * 8 + nh], in_=sml[:, 16:16 + nh],
                                                    func=AF.Exp, scale=-0.5), ["sml2"], [("srs", b)])
                R.add("dve", lambda e: e.tensor_tensor(
                    out=qn[b][:, 0:nh * 128].rearrange("p (h d) -> p h d", d=128),
                    in0=srcv.rearrange("p (h d) -> p h d", d=128),
                    in1=sml[:, 32 + b * 8:32 + b * 8 + nh].unsqueeze(2).to_broadcast([128, nh, 128]), op=ALU.mult),
                    stoks + [("srs", b)], [("qn", b)])
                tp = ps[:, b, :].bitcast(BF16)
                for c in range(nh):
                    R.add("pe", lambda e, c=c: e.transpose(out=tp[:, c * 128:(c + 1) * 128],
                                                           in_=qn[b][:, c * 128:(c + 1) * 128], identity=identb[:, :]),
                          [("qn", b), "identb"], [("ps", b)])
                R.add("dve", lambda e: e.tensor_scalar(
                    out=dst, in0=tp[:, 0:nh * 128].rearrange("p (h n) -> p h n", h=nh),
                    scalar1=cst[:, gcolumn:gcolumn + 1], scalar2=scale, op0=ALU.mult, op1=ALU.mult),
                    [("ps", b)], [dtok])

            for mt in range(2):
                pb = 4 + mt * 2
                for half in range(2):
                    k, wv = kvs[half]
                    for c in range(8):
                        R.add("pe", lambda e, c=c, wv=wv, half=half, mt=mt, pb=pb: e.matmul(
                            ps[:, pb + half, :], lhsT=hmT[:, c, mt * 128:(mt + 1) * 128], rhs=wv[:, c, :],
                            start=(c == 0), stop=(c == 7)), [("hmT", mt), ("w", k, 0)], [("ps", pb + half)])
                head_norm_T(pb, ps[:, pb, :], mt, 4, ccol(l, C_XK), 1.0, kTx[:, :, mt * 128:(mt + 1) * 128],
                            ("kTx", mt), [("ps", pb)])
                R.add("dve", lambda e, mt=mt, pb=pb: e.tensor_copy(
                    out=vx[:, mt, :, 0:128], in_=ps[:, pb + 1, :].rearrange("p (h d) -> p h d", h=4)),
                    [("ps", pb + 1)], [("vx", mt)])
            for t in range(NT):
                b = t % 2
                pb = 4 + b
                for c in range(8):
                    R.add("pe", lambda e, c=c, t=t, pb=pb: e.matmul(
                        ps[:, pb, :], lhsT=hT[:, c, t * 128:(t + 1) * 128], rhs=wq_v[:, c, :], start=(c == 0),
                        stop=(c == 7)), [("hT", t), ("w", kwq, 0)], [("ps", pb)])
                head_norm_T(pb, ps[:, pb, :], b, 4, ccol(l, C_XQ), 128 ** -0.5, qTx[:, :, t * 128:(t + 1) * 128],
                            ("qTx", t), [("ps", pb)])
            it = 0
            for qg in range(4):
                xb = qg % 2
                for h in range(4):
                    ob = 4 + (h % 2) * 2
                    slots = []
                    for kt in range(2):
                        sbk = it % 4
                        it += 1
                        slots.append(sbk)
                        zb = 2 + (sbk % 2)
                        R.add("pe", lambda e, h=h, kt=kt, qg=qg, zb=zb: e.matmul(
                            ps[:, zb, :], lhsT=kTx[:, h, kt * 128:(kt + 1) * 128],
                            rhs=qTx[:, h, qg * 512:(qg + 1) * 512], start=True, stop=True),
                            [("kTx", kt)] + [("qTx", qg * 4 + i) for i in range(4)], [("ps", zb)])
                        R.add("act", lambda e, zb=zb, sbk=sbk: e.activation(out=Pb[sbk][:, :], in_=ps[:, zb, :],
                                                                            func=AF.Exp), [("ps", zb)], [("P", sbk)])
                    for jj in range(4):
                        for kt in range(2):
                            sbk = slots[kt]
                            R.add("pe", lambda e, jj=jj, sbk=sbk, kt=kt, h=h, ob=ob: e.matmul(
                                ps[:, ob + jj // 2, (jj % 2) * 129:(jj % 2 + 1) * 129],
                                lhsT=Pb[sbk][:, jj * 128:(jj + 1) * 128], rhs=vx[:, kt, h, :], start=(kt == 0),
                                stop=(kt == 1)), [("P", sbk), ("vx", kt), "vx1"], [("ps", ob + jj // 2)])
                    ov = ps[:, ob:ob + 2, 0:258].rearrange("p a (j d) -> p a j d", j=2)
                    R.add("dve", lambda e, ov=ov: e.reciprocal(out=sml[:, 48:52].rearrange("p (a j) -> p a j", a=2),
                                                               in_=ov[:, :, :, 128]),
                          [("ps", ob), ("ps", ob + 1)], ["rec"])
                    for a2 in range(2):
                        R.add("dve", lambda e, ov=ov, a2=a2, h=h, xb=xb: e.tensor_tensor(
                            out=xor_[xb][:, a2 * 2:(a2 + 1) * 2, h * 128:(h + 1) * 128], in0=ov[:, a2, :, 0:128],
                            in1=sml[:, 48 + a2 * 2:50 + a2 * 2].unsqueeze(2).to_broadcast([128, 2, 128]), op=ALU.mult),
                            [("ps", ob + a2), "rec"], [("xor", xb)])
                for jj in range(4):
                    t = qg * 4 + jj
                    b = t % 2
                    tp = ps[:, b, :].bitcast(BF16)
                    for c in range(4):
                        R.add("pe", lambda e, c=c, jj=jj, xb=xb, tp=tp: e.transpose(
                            out=tp[:, c * 128:(c + 1) * 128], in_=xor_[xb][:, jj, c * 128:(c + 1) * 128],
                            identity=identb[:, :]), [("xor", xb), "identb"], [("ps", b)])
                    R.add("act", lambda e, t=t, tp=tp: e.activation(
                        out=hT[:, 0:4, t * 128:(t + 1) * 128], in_=tp[:, 0:512].rearrange("p (c n) -> p c n", c=4),
                        func=AF.Copy), [("ps", b)], [("hT", t)])
            out_proj(wd_["xattn_wo"][l], 4)
            barrier()

        def ffn(l):
            i = l // 2
            moe = (l % 2 == 1)
            A0 = 6 * KB
            actT = avb(A0, 8192).rearrange("p (j n) -> p j n", j=4)
            sg = [av(A0 + 16 * KB + k * 2 * KB, 2 * KB) for k in range(2)]
            hn32 = av(A0 + 20 * KB, 4 * KB)
            h32T = av(A0 + 24 * KB, 4 * KB).rearrange("p (c n) -> p c n", c=8)
            gates = av(A0 + 28 * KB, 512).rearrange("p (t e) -> p t e", t=16)
            sm = av(A0 + 29 * KB, 512)

            def router_tile(t, b, src, stok):
                R.add("dve", lambda e: e.tensor_scalar(out=hn32[:, :], in0=src, scalar1=stat[:, 16 + b * 4:17 + b * 4],
                                                       scalar2=None, op0=ALU.mult), [stok, ("rstd", b)], ["hn32"])
                tpf = ps[:, 2:4, :].rearrange("p a n -> p (a n)")
                for c in range(8):
                    R.add("pe", lambda e, c=c: e.transpose(out=tpf[:, c * 128:(c + 1) * 128],
                                                           in_=hn32[:, c * 128:(c + 1) * 128], identity=identf[:, :]),
                          ["hn32", "identf"], [("ps", 2), ("ps", 3)])
                gc = ccol(l, C_GFFN)
                R.add("dve", lambda e: e.tensor_tensor(
                    out=h32T, in0=tpf.rearrange("p (c n) -> p c n", c=8),
                    in1=cst[:, gc:gc + 8].unsqueeze(2).to_broadcast([128, 8, 128]), op=ALU.mult),
                    [("ps", 2), ("ps", 3)], ["h32T"])
                for c in range(8):
                    R.add("pe", lambda e, c=c: e.matmul(ps[:, 4, 0:NE], lhsT=h32T[:, c, :], rhs=rw[:, i, c, :],
                                                        start=(c == 0), stop=(c == 7)), ["h32T"], [("ps", 4)])
                lg, eq1, l2, eq2 = sm[:, 0:8], sm[:, 8:16], sm[:, 16:24], sm[:, 24:32]
                m1, m2, dd, ee, g1, g2 = (sm[:, 32:33], sm[:, 33:34], sm[:, 34:35], sm[:, 35:36], sm[:, 36:37],
                                          sm[:, 37:38])
                ga = sm[:, 40:48]
                dv = lambda fn, r, w: R.add("dve", fn, r, w)
                dv(lambda e: e.tensor_tensor(out=lg, in0=ps[:, 4, 0:NE], in1=cst[:, C_RB + i * 8:C_RB + i * 8 + 8],
                                             op=ALU.add), [("ps", 4)], ["r0"])
                dv(lambda e: e.tensor_reduce(out=m1, in_=lg, axis=AX.X, op=ALU.max), ["r0"], ["r1"])
                dv(lambda e: e.tensor_scalar(out=eq1, in0=lg, scalar1=m1, scalar2=None, op0=ALU.is_equal),
                   ["r0", "r1"], ["r2"])
                dv(lambda e: e.scalar_tensor_tensor(out=l2, in0=eq1, scalar=-1e30, in1=lg, op0=ALU.mult, op1=ALU.add),
                   ["r2", "r0"], ["r3"])
                dv(lambda e: e.tensor_reduce(out=m2, in_=l2, axis=AX.X, op=ALU.max), ["r3"], ["r4"])
                dv(lambda e: e.tensor_scalar(out=eq2, in0=l2, scalar1=m2, scalar2=None, op0=ALU.is_equal),
                   ["r3", "r4"], ["r5"])
                dv(lambda e: e.tensor_tensor(out=dd, in0=m2, in1=m1, op=ALU.subtract), ["r4", "r1"], ["r6"])
                R.add("act", lambda e: e.activation(out=ee, in_=dd, func=AF.Exp), ["r6"], ["r7"])
                dv(lambda e: e.tensor_scalar(out=g1, in0=ee, scalar1=1.0, scalar2=None, op0=ALU.add), ["r7"], ["r8"])
                dv(lambda e: e.reciprocal(out=g1, in_=g1), ["r8"], ["r8"])
                dv(lambda e: e.tensor_tensor(out=g2, in0=ee, in1=g1, op=ALU.mult), ["r7", "r8"], ["r9"])
                dv(lambda e: e.tensor_scalar(out=ga, in0=eq1, scalar1=g1, scalar2=None, op0=ALU.mult),
                   ["r2", "r8"], ["r10"])
                dv(lambda e: e.scalar_tensor_tensor(out=gates[:, t, :], in0=eq2, scalar=g2, in1=ga, op0=ALU.mult,
                                                    op1=ALU.add), ["r5", "r9", "r10"], ["gates"])

            x_norm_T(ccol(l, C_GFFN), router_tile if moe else None)

            def ffn_weights(ei):
                if moe:
                    return wd_["exp_w_gate"][i, ei], wd_["exp_w_up"][i, ei], wd_["exp_w_down"][i, ei]
                return wd_["dense_w_gate"][i], wd_["dense_w_up"][i], wd_["dense_w_down"][i]

            for ei in range(NE if moe else 1):
                wg_d, wu_d, wdn_d = ffn_weights(ei)
                for fg in range(7):
                    f0 = fg * 512
                    kg, ku, kd = wslot(), wslot(), wslot()
                    wgv = wpool[:, kg, :].rearrange("p (c n) -> p c n", c=8)
                    wuv = wpool[:, ku, :].rearrange("p (c n) -> p c n", c=8)
                    wdv = wpool[:, kd, :].rearrange("p (j n) -> p j n", j=4)
                    R.dma("pool", wgv, wg_d[:, f0:f0 + 512].rearrange("(c p) f -> p c f", p=128), [], [("w", kg, 0)],
                          key=("w", kg, 0))
                    R.dma("pool", wuv, wu_d[:, f0:f0 + 512].rearrange("(c p) f -> p c f", p=128), [], [("w", ku, 0)],
                          key=("w", ku, 0))
                    R.dma("pool", wdv, wdn_d[f0:f0 + 512, :].rearrange("(j p) d -> p j d", p=128), [], [("w", kd, 0)],
                          key=("w", kd, 0))
                    kk = 0
                    for tg in range(4):
                        hr = [("hT", tg * 4 + q) for q in range(4)]
                        for j in range(4):
                            pb = (kk % 2) * 2
                            sb_ = kk % 2
                            kk += 1
                            for c in range(8):
                                R.add("pe", lambda e, c=c, j=j, tg=tg, pb=pb, wgv=wgv: e.matmul(
                                    ps[:, pb, :], lhsT=wgv[:, c, j * 128:(j + 1) * 128],
                                    rhs=hT[:, c, tg * 512:(tg + 1) * 512], start=(c == 0), stop=(c == 7)),
                                    hr + [("w", kg, 0)], [("ps", pb)])
                            for c in range(8):
                                R.add("pe", lambda e, c=c, j=j, tg=tg, pb=pb, wuv=wuv: e.matmul(
                                    ps[:, pb + 1, :], lhsT=wuv[:, c, j * 128:(j + 1) * 128],
                                    rhs=hT[:, c, tg * 512:(tg + 1) * 512], start=(c == 0), stop=(c == 7)),
                                    hr + [("w", ku, 0)], [("ps", pb + 1)])
                            R.add("act", lambda e, pb=pb, sb_=sb_: e.activation(out=sg[sb_][:, :], in_=ps[:, pb, :],
                                                                                func=AF.Silu),
                                  [("ps", pb)], [("sg", sb_)])
                            R.add("dve", lambda e, pb=pb, sb_=sb_, j=j, tg=tg: e.tensor_tensor(
                                out=actT[:, j, tg * 512:(tg + 1) * 512], in0=sg[sb_][:, :], in1=ps[:, pb + 1, :],
                                op=ALU.mult), [("sg", sb_), ("ps", pb + 1)], [("actT", tg)])
                    for t in range(NT):
                        pb = 4 + (t % 2) * 2
                        for half in range(2):
                            for j in range(4):
                                R.add("pe", lambda e, t=t, j=j, half=half, pb=pb, wdv=wdv: e.matmul(
                                    ps[:, pb + half, :], lhsT=actT[:, j, t * 128:(t + 1) * 128],
                                    rhs=wdv[:, j, half * 512:(half + 1) * 512], start=(j == 0), stop=(j == 3)),
                                    [("actT", t // 4), ("w", kd, 0)], [("ps", pb + half)])
                        add_to_x(t, pb, gates[:, t, ei:ei + 1] if moe else None)
            barrier()

        for s in range(nseq):
            for t in range(NT):
                R.dma("sp", x[:, t, :], x_d[s, t * 128:(t + 1) * 128, :], [], [("x", t)], key=("x", t))
            for l in layers:
                if "mix" in phases or "sb" in phases or "swa" in phases:
                    mixer(l)
                if "xat" in phases:
                    xattn(l, s)
                if "ffn" in phases:
                    ffn(l)
            for t in range(NT):
                R.dma("sp", y_d[s, t * 128:(t + 1) * 128, :], x[:, t, :], [("x", t)], [("y", s, t)], key=("x", t))
        R.add("sp", None, [("y", s, t) for s in range(nseq) for t in range(NT)], [])
        nsem = R.emit(nc)
        build_program.info = (R.nops, nsem)
    return nc


def _t5_buckets(dist):
    n = np.maximum(dist, 0)
    max_exact = 16
    large = max_exact + (np.log(np.maximum(n, 1) / max_exact) / np.log(128 / max_exact) * (32 - max_exact)).astype(
        np.int32)
    large = np.minimum(large, 31)
    return np.where(n < max_exact, n, large).astype(np.int32)


def prep_shared(inputs):
    rel_bias = np.asarray(inputs["rel_bias"], dtype=np.float32)
    dist = 128 + np.arange(128)[:, None] - np.arange(256)[None, :]
    bt = rel_bias[_t5_buckets(dist)]
    biast = np.ascontiguousarray(np.transpose(bt, (1, 2, 0)))
    w_in = np.array(inputs["w_in"], dtype=np.float32, copy=True)
    q = w_in[:, :, 1536:2048].reshape(DEPTH, D, 2, 4, 64)
    w_in[:, :, 1536:2048] = np.transpose(q, (0, 1, 3, 2, 4)).reshape(DEPTH, D, 512)
    shared = {n: np.ascontiguousarray(np.asarray(inputs[n], dtype=np.float32)) for n in W_NAMES if n != "w_in"}
    shared["w_in"] = w_in
    shared["swa_bias_t"] = biast
    return shared


_CACHE = {}


def kernel(**inputs):
    n_cores = 8
    x = np.asarray(inputs["x"], dtype=np.float32)
    mem = np.asarray(inputs["mem"], dtype=np.float32)
    B = x.shape[0]
    nseq = B // n_cores
    if "nc" not in _CACHE:
        _CACHE["nc"] = build_program(nseq=nseq)
    nc = _CACHE["nc"]
    shared = prep_shared(inputs)
    in_maps = []
    for c in range(n_cores):
        m = dict(shared)
        m["x"] = np.ascontiguousarray(x[c * nseq:(c + 1) * nseq])
        m["mem"] = np.ascontiguousarray(mem[c * nseq:(c + 1) * nseq])
        in_maps.append(m)
    res = run_bass_kernel_spmd(nc, in_maps, core_ids=list(range(n_cores)))
    return np.concatenate([r["y"] for r in res.results], axis=0)
```

```python
import contextlib
import numpy as np
import concourse.bass as bass
import concourse.mybir as mybir
from concourse.bass_utils import run_bass_kernel_spmd

F32 = mybir.dt.float32
BF16 = mybir.dt.bfloat16
AF = mybir.ActivationFunctionType
ALU = mybir.AluOpType
AX = mybir.AxisListType

S = 2048
D = 1024
NT = 16
DEPTH = 4
DFF = 3584
NE = 8
NM = 256
EPS = 1e-6
NEG = -30000.0


class Op:
    __slots__ = ("eng", "fn", "deps", "dma_key", "idx", "inc", "val")


class Rec:
    ENGS = ("pe", "act", "dve", "pool", "sp")

    def __init__(self):
        self.ops = {e: [] for e in self.ENGS}
        self.lastw = {}
        self.readers = {}
        self.dma_cnt = {}
        self.last_dma = {}
        self.floor = None
        self.nops = 0

    @staticmethod
    def _key(d):
        if d.dma_key is not None:
            return ("dma", d.dma_key)
        return ("eng", d.eng)

    def add(self, eng, fn, reads=(), writes=(), dma_key=None, extra=()):
        op = Op()
        op.eng = eng
        op.fn = fn
        op.dma_key = dma_key
        op.inc = dma_key is not None
        deps = {}

        def adddep(d):
            if d.dma_key is None and d.eng == "pe" and eng == "pe" and dma_key is None:
                return
            k = self._key(d)
            v = d.val if d.dma_key is not None else d.idx
            cur = deps.get(k)
            if cur is None or cur[0] < v:
                deps[k] = (v, d)

        if self.floor is not None:
            adddep(self.floor)
        for d in extra:
            adddep(d)
        for t in reads:
            d = self.lastw.get(t)
            if d is not None:
                adddep(d)
        for t in writes:
            d = self.lastw.get(t)
            if d is not None:
                adddep(d)
            rs = self.readers.get(t)
            if rs:
                for (_, r) in rs.values():
                    adddep(r)
        lst = self.ops[eng]
        op.idx = len(lst)
        if dma_key is not None:
            c = self.dma_cnt.get(dma_key, 0) + 16
            self.dma_cnt[dma_key] = c
            op.val = c
            self.last_dma[dma_key] = op
        else:
            op.val = None
        op.deps = [d for (_, d) in deps.values()]
        for d in op.deps:
            d.inc = True
        lst.append(op)
        self.nops += 1
        for t in writes:
            self.lastw[t] = op
            self.readers[t] = {}
        k = self._key(op)
        v = op.val if dma_key is not None else op.idx
        for t in reads:
            rs = self.readers.get(t)
            if rs is None:
                rs = self.readers[t] = {}
            rs[k] = (v, op)
        return op

    def barrier(self, fn):
        extra = []
        for e in ("pe", "act", "dve", "pool"):
            for op in reversed(self.ops[e]):
                if op.dma_key is None and op.fn is not None:
                    extra.append(op)
                    break
        extra.extend(self.last_dma.values())
        b = self.add("pool", fn, (), (), extra=extra)
        self.floor = b
        return b

    def dma(self, eng, out, in_, reads, writes, key, **kw):
        return self.add(eng, lambda e: e.dma_start(out=out, in_=in_, **kw), reads, writes, dma_key=key)

    def emit(self, nc):
        for e in self.ENGS:
            c = 0
            for op in self.ops[e]:
                if op.dma_key is None:
                    if op.inc:
                        c += 1
                    op.val = c
        with contextlib.ExitStack() as st:
            sems = {}
            for e in ("pe", "act", "dve", "pool"):
                sems[("eng", e)] = st.enter_context(nc.semaphore("s_" + e))
            for i, k in enumerate(self.dma_cnt.keys()):
                sems[("dma", k)] = st.enter_context(nc.semaphore("d%d" % i))
            block = st.enter_context(nc.Block())

            def replay(ename):
                def body(eng):
                    waited = {}
                    for op in self.ops[ename]:
                        for d in op.deps:
                            k = self._key(d)
                            if waited.get(k, 0) < d.val:
                                eng.wait_ge(sems[k], d.val)
                                waited[k] = d.val
                        if op.fn is None:
                            continue
                        ins = op.fn(eng)
                        if op.dma_key is not None:
                            ins.then_inc(sems[("dma", op.dma_key)], 16)
                        elif op.inc:
                            ins.then_inc(sems[("eng", ename)], 1)
                return body

            block.tensor(replay("pe"))
            block.scalar(replay("act"))
            block.vector(replay("dve"))
            block.gpsimd(replay("pool"))
            block.sync(replay("sp"))
        return len(self.dma_cnt)


W_NAMES = ["norm_mix", "w_in", "sb_out_gain", "swa_q_gain", "swa_k_gain", "swa_sinks", "swa_out_gain",
           "w_out", "norm_xattn", "norm_mem", "xattn_wq", "xattn_wkv", "xattn_q_gain", "xattn_k_gain",
           "xattn_wo", "norm_ffn", "dense_w_gate", "dense_w_up", "dense_w_down", "router_w", "router_b",
           "exp_w_gate", "exp_w_up", "exp_w_down"]
W_SHAPES = {
    "norm_mix": [DEPTH, D], "w_in": [DEPTH, D, 2304], "sb_out_gain": [DEPTH, 512], "swa_q_gain": [DEPTH, 64],
    "swa_k_gain": [DEPTH, 64], "swa_sinks": [DEPTH, 8], "swa_out_gain": [DEPTH, 512], "w_out": [DEPTH, D, D],
    "norm_xattn": [DEPTH, D], "norm_mem": [DEPTH, D], "xattn_wq": [DEPTH, D, 512], "xattn_wkv": [DEPTH, D, 1024],
    "xattn_q_gain": [DEPTH, 128], "xattn_k_gain": [DEPTH, 128], "xattn_wo": [DEPTH, 512, D], "norm_ffn": [DEPTH, D],
    "dense_w_gate": [2, D, DFF], "dense_w_up": [2, D, DFF], "dense_w_down": [2, DFF, D], "router_w": [2, D, NE],
    "router_b": [2, NE], "exp_w_gate": [2, NE, D, DFF], "exp_w_up": [2, NE, D, DFF], "exp_w_down": [2, NE, DFF, D],
}


def build_program(nseq=4, layers=(0, 1, 2, 3), phases=("mix", "xat", "ffn")):
    nc = bass.Bass("TRN2", target_bir_lowering=False)
    x_d = nc.dram_tensor("x", [nseq, S, D], F32, kind="ExternalInput").ap()
    mem_d = nc.dram_tensor("mem", [nseq, NM, D], F32, kind="ExternalInput").ap()
    biast_d = nc.dram_tensor("swa_bias_t", [256, 8, 128], F32, kind="ExternalInput").ap()
    wd_ = {n: nc.dram_tensor(n, W_SHAPES[n], F32, kind="ExternalInput").ap() for n in W_NAMES}
    y_d = nc.dram_tensor("y", [nseq, S, D], F32, kind="ExternalOutput").ap()
    mixed_d = nc.dram_tensor("mixed_scr", [S, D], BF16).ap()
    R = Rec()

    with contextlib.ExitStack() as st:
        x = st.enter_context(nc.sbuf_tensor("sb_x", [128, NT, D], F32))
        hT = st.enter_context(nc.sbuf_tensor("sb_hT", [128, 8, S], BF16))
        wpool = st.enter_context(nc.sbuf_tensor("sb_w", [128, 6, 4096], BF16))
        arena = st.enter_context(nc.sbuf_tensor("sb_arena", [128, 14336], F32))
        cst = st.enter_context(nc.sbuf_tensor("sb_cst", [128, 320], F32))
        rw = st.enter_context(nc.sbuf_tensor("sb_rw", [128, 2, 8, NE], F32))
        identf = st.enter_context(nc.sbuf_tensor("sb_identf", [128, 128], F32))
        identb = st.enter_context(nc.sbuf_tensor("sb_identb", [128, 128], BF16))
        negtri = st.enter_context(nc.sbuf_tensor("sb_negtri", [128, 128], BF16))
        onesb = st.enter_context(nc.sbuf_tensor("sb_ones", [128, 128], BF16))
        maskd = st.enter_context(nc.sbuf_tensor("sb_maskd", [128, 128], BF16))
        tmpf = st.enter_context(nc.sbuf_tensor("sb_tmpf", [128, 128], F32))
        dummy = st.enter_context(nc.sbuf_tensor("sb_dummy", [128, 2], F32))
        stat = st.enter_context(nc.sbuf_tensor("sb_stat", [128, 64], F32))
        ps = st.enter_context(nc.psum_tensor("ps", [128, 8, 512], F32))

        def av(off, nbytes):
            assert off % 4 == 0 and nbytes % 4 == 0 and off + nbytes <= 57344, (off, nbytes)
            return arena[:, off // 4:(off + nbytes) // 4]

        def avb(off, nelem):
            return av(off, nelem * 2).bitcast(BF16)

        KB = 1024
        hn = [avb(0, 1024), avb(2 * KB, 1024)]
        junk = avb(4 * KB, 1024)

        def ccol(l, k):
            return l * 48 + k
        C_GMIX, C_GXAT, C_GFFN, C_GOUT, C_GMEM, C_SWQ, C_SWK, C_XQ, C_XK = 0, 8, 16, 24, 32, 40, 41, 42, 43
        C_ESINK = 192
        C_RB = 224

        bar_n = [0]

        def barrier():
            bar_n[0] += 1
            R.barrier(lambda e: e.memset(dummy[:, 0:1], 0.0))

        def cdma(out, in_, tok, **kw):
            R.dma("sp", out, in_, [], [tok], key=("c", tok), **kw)

        cidx = [0]

        def cload(out, in_, **kw):
            cidx[0] += 1
            R.dma("sp", out, in_, [], [("cst", cidx[0])], key=("c", cidx[0] % 4), **kw)

        def gT_load(col, vec):
            n = vec.shape[0] // 128
            cload(cst[:, col:col + n], vec.rearrange("(c p) -> p c", p=128), allow_slow_non_contiguous=True)

        for l in range(DEPTH):
            gT_load(ccol(l, C_GMIX), wd_["norm_mix"][l])
            gT_load(ccol(l, C_GXAT), wd_["norm_xattn"][l])
            gT_load(ccol(l, C_GFFN), wd_["norm_ffn"][l])
            gT_load(ccol(l, C_GOUT), wd_["sb_out_gain"][l])
            gT_load(ccol(l, C_GOUT) + 4, wd_["swa_out_gain"][l])
            gT_load(ccol(l, C_GMEM), wd_["norm_mem"][l])
            gT_load(ccol(l, C_XQ), wd_["xattn_q_gain"][l])
            gT_load(ccol(l, C_XK), wd_["xattn_k_gain"][l])
            for half in range(2):
                cload(cst[half * 64:(half + 1) * 64, ccol(l, C_SWQ):ccol(l, C_SWQ) + 1],
                      wd_["swa_q_gain"][l].rearrange("(d o) -> d o", o=1), allow_slow_non_contiguous=True)
                cload(cst[half * 64:(half + 1) * 64, ccol(l, C_SWK):ccol(l, C_SWK) + 1],
                      wd_["swa_k_gain"][l].rearrange("(d o) -> d o", o=1), allow_slow_non_contiguous=True)
        cload(cst[:, C_ESINK:C_ESINK + 32], wd_["swa_sinks"].rearrange("l h -> (l h)").partition_broadcast(128))
        cload(cst[:, C_RB:C_RB + 16], wd_["router_b"].rearrange("i e -> (i e)").partition_broadcast(128))
        for i in range(2):
            cload(rw[:, i, :, :], wd_["router_w"][i].rearrange("(c p) e -> p c e", p=128),
                  allow_slow_non_contiguous=True)
        R.add("pool", lambda e: e.memset(tmpf[:, :], 1.0), [], ["tmpf"])
        R.add("pool", lambda e: e.affine_select(out=identf[:, :], in_=tmpf[:, :], pattern=[[-1, 128]],
                                                compare_op=ALU.is_equal, fill=0.0, base=0, channel_multiplier=1),
              ["tmpf"], ["identf"])
        R.add("pool", lambda e: e.tensor_copy(out=identb[:, :], in_=identf[:, :]), ["identf"], ["identb"])
        R.add("pool", lambda e: e.tensor_copy(out=onesb[:, :], in_=tmpf[:, :]), ["tmpf"], ["onesb"])
        R.add("pool", lambda e: e.affine_select(out=maskd[:, :], in_=tmpf[:, :], pattern=[[1, 128]],
                                                compare_op=ALU.is_gt, fill=0.0, base=0, channel_multiplier=-1),
              ["tmpf"], ["maskd"])
        R.add("pool", lambda e: e.memset(tmpf[:, :], -1.0), ["tmpf"], ["tmpf"])
        R.add("pool", lambda e: e.affine_select(out=negtri[:, :], in_=tmpf[:, :], pattern=[[-1, 128]],
                                                compare_op=ALU.is_ge, fill=0.0, base=0, channel_multiplier=1),
              ["tmpf"], ["negtri"])
        barrier()
        R.add("act", lambda e: e.activation(out=cst[:, C_ESINK:C_ESINK + 32], in_=cst[:, C_ESINK:C_ESINK + 32],
                                            func=AF.Exp), [], ["esink"])
        barrier()

        wctr = [0]

        def wslot():
            k = wctr[0] % 6
            wctr[0] += 1
            return k

        def wload(k, part, col0, ncols, src):
            raise NotImplementedError

        def norm_T(src_fn, src_tok_fn, ntiles, groups, gcol, dst_fn, dst_tok_fn, extra_tile_fn=None):
            ng = len(groups)
            W = groups[-1][1]
            nch = W // 128
            for t in range(ntiles):
                b = t % 2
                src = src_fn(t)
                stok = src_tok_fn(t)
                ssv = stat[:, 0:ng]
                for gi, (c0, c1) in enumerate(groups):
                    R.add("act", lambda e, src=src, c0=c0, c1=c1, gi=gi: e.activation(
                        out=junk[:, c0:c1], in_=src[:, c0:c1], func=AF.Square, accum_out=stat[:, gi:gi + 1]),
                        [stok], ["junk", "stat"])
                wdt = groups[0][1] - groups[0][0]
                R.add("act", lambda e, ssv=ssv: e.activation(out=stat[:, 8:8 + ng], in_=ssv, func=AF.Ln, bias=EPS,
                                                            scale=1.0 / wdt), ["stat"], ["stat2"])
                R.add("act", lambda e, b=b: e.activation(out=stat[:, 16 + b * 4:16 + b * 4 + ng],
                                                         in_=stat[:, 8:8 + ng], func=AF.Exp, scale=-0.5),
                      ["stat2"], [("rstd", b)])
                for gi, (c0, c1) in enumerate(groups):
                    R.add("dve", lambda e, src=src, c0=c0, c1=c1, gi=gi, b=b: e.tensor_scalar(
                        out=hn[b][:, c0:c1], in0=src[:, c0:c1], scalar1=stat[:, 16 + b * 4 + gi:17 + b * 4 + gi],
                        scalar2=None, op0=ALU.mult), [stok, ("rstd", b)], [("hn", b)])
                if extra_tile_fn is not None:
                    extra_tile_fn(t, b, src, stok)
                tp = ps[:, b, :].bitcast(BF16)
                for c in range(nch):
                    R.add("pe", lambda e, c=c, b=b, tp=tp: e.transpose(
                        out=tp[:, c * 128:(c + 1) * 128], in_=hn[b][:, c * 128:(c + 1) * 128], identity=identb[:, :]),
                        [("hn", b), "identb"], [("ps", b)])
                dst = dst_fn(t)
                R.add("dve", lambda e, tp=tp, dst=dst: e.tensor_tensor(
                    out=dst, in0=tp[:, 0:nch * 128].rearrange("p (c n) -> p c n", c=nch),
                    in1=cst[:, gcol:gcol + nch].unsqueeze(2).to_broadcast([128, nch, 128]), op=ALU.mult),
                    [("ps", b)], [dst_tok_fn(t)])

        def x_norm_T(gcol, extra_tile_fn=None):
            norm_T(lambda t: x[:, t, :], lambda t: ("x", t), NT, [(0, D)], gcol,
                   lambda t: hT[:, :, t * 128:(t + 1) * 128], lambda t: ("hT", t), extra_tile_fn)

        def add_to_x(t, pb, gate_ap=None):
            src = ps[:, pb:pb + 2, :].rearrange("p a n -> p (a n)")
            if gate_ap is None:
                R.add("dve", lambda e: e.tensor_tensor(out=x[:, t, :], in0=src, in1=x[:, t, :], op=ALU.add),
                      [("ps", pb), ("ps", pb + 1), ("x", t)], [("x", t)])
            else:
                R.add("dve", lambda e: e.scalar_tensor_tensor(out=x[:, t, :], in0=src, scalar=gate_ap, in1=x[:, t, :],
                                                              op0=ALU.mult, op1=ALU.add),
                      [("ps", pb), ("ps", pb + 1), ("x", t), "gates"], [("x", t)])

        def out_proj(wsrc, nch):
            ks = []
            for half in range(2):
                k = wslot()
                wv = wpool[:, k, 0:nch * 512].rearrange("p (c n) -> p c n", c=nch)
                R.dma("pool", wv, wsrc[:, half * 512:(half + 1) * 512].rearrange("(c p) n -> p c n", p=128),
                      [], [("w", k, 0)], key=("w", k, 0))
                ks.append((k, wv))
            for t in range(NT):
                pb = 4 + (t % 2) * 2
                for half in range(2):
                    k, wv = ks[half]
                    for c in range(nch):
                        R.add("pe", lambda e, c=c, half=half, wv=wv, pb=pb, t=t: e.matmul(
                            ps[:, pb + half, :], lhsT=hT[:, c, t * 128:(t + 1) * 128], rhs=wv[:, c, :],
                            start=(c == 0), stop=(c == nch - 1)), [("hT", t), ("w", k, 0)], [("ps", pb + half)])
                add_to_x(t, pb)

        def mixer(l):
            w_in = wd_["w_in"][l]
            x_norm_T(ccol(l, C_GMIX))
            PB = 6 * KB

            def pair_bufs(i):
                o = PB + i * 12 * KB
                return (avb(o, 2048), avb(o + 4 * KB, 2048),
                        avb(o + 8 * KB, 2048).rearrange("p (t d) -> p t d", t=16))
            SO = PB + 24 * KB
            ebuf = [av(SO + i * 2 * KB, 2 * KB) for i in range(2)]
            spb = [avb(SO + 4 * KB + i * KB, 512) for i in range(2)]
            lgA = [av(SO + 6 * KB + i * 2 * KB, 2 * KB) for i in range(2)]
            Ab = [avb(SO + 10 * KB + i * KB, 512) for i in range(2)]
            Rb = [av(SO + 12 * KB + i * 2 * KB, 2 * KB) for i in range(2)]
            ost = [avb(SO + 16 * KB + i * KB, 512).rearrange("p (t d) -> p t d", t=4) for i in range(2)]

            def sb_proj(p):
                pi = p % 2
                qT, kT, vv = pair_bufs(pi)
                k = wslot()
                wv = wpool[:, k, 0:8 * 384].rearrange("p (c n) -> p c n", c=8)
                for part in range(3):
                    c0 = part * 512 + p * 128
                    R.dma("pool", wv[:, :, part * 128:(part + 1) * 128],
                          w_in[:, c0:c0 + 128].rearrange("(c p) n -> p c n", p=128), [], [("w", k, part)],
                          key=("w", k, part))
                gi = 0
                for part, dst in ((0, qT), (1, kT)):
                    for tg in range(4):
                        bk = 7 if gi % 2 == 0 else 4
                        gi += 1
                        for c in range(8):
                            R.add("pe", lambda e, c=c, tg=tg, part=part, wv=wv, bk=bk: e.matmul(
                                ps[:, bk, :], lhsT=wv[:, c, part * 128:(part + 1) * 128],
                                rhs=hT[:, c, tg * 512:(tg + 1) * 512], start=(c == 0), stop=(c == 7)),
                                [("hT", tg * 4 + i) for i in range(4)] + [("w", k, part)], [("ps", bk)])
                        sc = 0.125 if part == 0 else 1.0
                        R.add("act", lambda e, dst=dst, tg=tg, sc=sc, bk=bk: e.activation(
                            out=dst[:, tg * 512:(tg + 1) * 512], in_=ps[:, bk, :], func=AF.Copy, scale=sc),
                            [("ps", bk)], [("pq", pi, part, tg)])
                for t4 in range(4):
                    bk = 7 if gi % 2 == 0 else 4
                    gi += 1
                    for tt in range(4):
                        t = t4 * 4 + tt
                        for c in range(8):
                            R.add("pe", lambda e, c=c, t=t, tt=tt, wv=wv, bk=bk: e.matmul(
                                ps[:, bk, tt * 128:(tt + 1) * 128], lhsT=hT[:, c, t * 128:(t + 1) * 128],
                                rhs=wv[:, c, 256:384], start=(c == 0), stop=(c == 7)),
                                [("hT", t), ("w", k, 2)], [("ps", bk)])
                    R.add("dve", lambda e, vv=vv, t4=t4, bk=bk: e.tensor_copy(
                        out=vv[:, t4 * 4:(t4 + 1) * 4, :], in_=ps[:, bk, :].rearrange("p (t d) -> p t d", t=4)),
                        [("ps", bk)], [("pv", pi, t4)])

            def sb_attn(p):
                pi = p % 2
                qT, kT, vv = pair_bufs(pi)
                items = []
                for g in range(4):
                    for hp in range(2):
                        for a in range(4 * g + 3, -1, -1):
                            items.append((g, hp, a))
                ucount = [0]

                def unit_of(g, hp):
                    return g * 2 + hp

                def cols(g, a):
                    j = max(a - 4 * g, 0)
                    return j * 128, 512 - j * 128

                def st1(it, k):
                    g, hp, a = it
                    c0, n = cols(g, a)
                    pb = hp * 64
                    zb = k % 2
                    R.add("pe", lambda e: e.matmul(ps[:, zb, 0:n], lhsT=kT[pb:pb + 64, a * 128:(a + 1) * 128],
                                                   rhs=qT[pb:pb + 64, g * 512 + c0:(g + 1) * 512], start=True,
                                                   stop=True),
                          [("pq", pi, 0, g), ("pq", pi, 1, a // 4)], [("ps", zb)])
                    R.add("act", lambda e: e.activation(out=ebuf[zb][:, 0:n], in_=ps[:, zb, 0:n], func=AF.Exp),
                          [("ps", zb)], [("e", zb)])

                def st1b(it, k):
                    g, hp, a = it
                    c0, n = cols(g, a)
                    zb = k % 2
                    R.add("act", lambda e: e.activation(out=spb[zb][:, 0:n], in_=ebuf[zb][:, 0:n], func=AF.Ln,
                                                        bias=1.0), [("e", zb)], [("sp", zb)])
                    if a >= 4 * g:
                        R.add("pool", lambda e: e.tensor_tensor(out=spb[zb][:, 0:128], in0=spb[zb][:, 0:128],
                                                                in1=maskd[:, :], op=ALU.mult),
                              [("sp", zb), "maskd"], [("sp", zb)])

                def st2a(it, k):
                    g, hp, a = it
                    c0, n = cols(g, a)
                    pb = hp * 64
                    zb = k % 2
                    u = unit_of(g, hp) % 2
                    if a == 4 * g + 3:
                        R.add("pool", lambda e: e.memset(Rb[u][:, :], 0.0), [], [("R", u)])
                        R.add("dve", lambda e: e.memset(ps[:, 5 + u, 0:256], 0.0), [], [("ps", 5 + u)])
                    R.add("pe", lambda e: e.matmul(ps[:, 2 + zb, 0:n], lhsT=kT[pb:pb + 64, a * 128:(a + 1) * 128],
                                                   rhs=qT[pb:pb + 64, g * 512 + c0:(g + 1) * 512], start=True,
                                                   stop=False),
                          [("pq", pi, 0, g), ("pq", pi, 1, a // 4)], [("ps", 2 + zb)])
                    R.add("pe", lambda e: e.matmul(ps[:, 2 + zb, 0:n], lhsT=negtri[:, :], rhs=spb[zb][:, 0:n],
                                                   start=False, stop=True), [("sp", zb), "negtri"], [("ps", 2 + zb)])
                    R.add("pe", lambda e: e.matmul(ps[:, 4, 0:n], lhsT=onesb[:, :], rhs=spb[zb][:, 0:n], start=True,
                                                   stop=True), [("sp", zb), "onesb"], [("ps", 4)])
                    R.add("dve", lambda e: e.tensor_tensor(out=lgA[zb][:, 0:n], in0=ps[:, 2 + zb, 0:n],
                                                           in1=Rb[u][:, c0:512], op=ALU.subtract),
                          [("ps", 2 + zb), ("R", u)], [("lgA", zb)])
                    R.add("dve", lambda e: e.tensor_tensor(out=Rb[u][:, c0:512], in0=ps[:, 4, 0:n],
                                                           in1=Rb[u][:, c0:512], op=ALU.add),
                          [("ps", 4), ("R", u)], [("R", u)])

                def st2b(it, k):
                    g, hp, a = it
                    c0, n = cols(g, a)
                    zb = k % 2
                    R.add("act", lambda e: e.activation(out=Ab[zb][:, 0:n], in_=lgA[zb][:, 0:n], func=AF.Exp),
                          [("lgA", zb)], [("A", zb)])
                    if a >= 4 * g:
                        R.add("pool", lambda e: e.tensor_tensor(out=Ab[zb][:, 0:128], in0=Ab[zb][:, 0:128],
                                                                in1=maskd[:, :], op=ALU.mult),
                              [("A", zb), "maskd"], [("A", zb)])

                def st3(it, k):
                    g, hp, a = it
                    c0, n = cols(g, a)
                    zb = k % 2
                    u = unit_of(g, hp) % 2
                    ob = 5 + u
                    j0 = c0 // 128
                    for jj in range(j0, 4):
                        tile = 4 * g + jj
                        R.add("pe", lambda e, jj=jj, tile=tile: e.matmul(
                            ps[:, ob, jj * 64:(jj + 1) * 64], lhsT=Ab[zb][:, (jj - j0) * 128:(jj - j0 + 1) * 128],
                            rhs=vv[:, a, hp * 64:(hp + 1) * 64], start=False, stop=(a == 0),
                            skip_group_check=True),
                            [("A", zb), ("pv", pi, a // 4)], [("ps", ob)])
                    if a == 0:
                        osb = g % 2
                        R.add("dve", lambda e: e.tensor_copy(
                            out=ost[osb][:, :, hp * 64:(hp + 1) * 64],
                            in_=ps[:, ob, 0:256].rearrange("p (t d) -> p t d", t=4)),
                            [("ps", ob)], [("ost", osb)])
                        if hp == 1:
                            R.dma("sp", mixed_d[g * 512:(g + 1) * 512, p * 128:(p + 1) * 128].rearrange(
                                "(t q) d -> q t d", q=128), ost[osb][:, :, :], [("ost", osb)],
                                [("mixed", g * 4 + i) for i in range(4)], key=("ost", osb))

                n_it = len(items)
                for k in range(n_it + 2):
                    if k < n_it:
                        st1(items[k], k)
                    if 2 <= k:
                        st2b(items[k - 2], k - 2)
                    if k < n_it:
                        st1b(items[k], k)
                    if 1 <= k <= n_it:
                        st2a(items[k - 1], k - 1)
                    if 2 <= k:
                        st3(items[k - 2], k - 2)

            if "sb" in phases or "mix" in phases:
                sb_proj(0)
                for p in range(4):
                    if p + 1 < 4:
                        sb_proj(p + 1)
                    sb_attn(p)
            barrier()
            A0 = 6 * KB
            qTs = avb(A0, 8192).rearrange("p (i n) -> p i n", i=4)
            kTs = avb(A0 + 16 * KB, 2048)
            vsw = avb(A0 + 20 * KB, 16 * 2 * 65 + 32)[:, 0:16 * 2 * 65].rearrange("p (t g d) -> p t g d", t=16, g=2)
            bia = av(A0 + 25 * KB, 8 * KB).rearrange("p (c h q) -> p c h q", c=2, h=8)
            sq = av(A0 + 33 * KB, 640 * 4)
            qn = [avb(A0 + 36 * KB + i * 1536, 640) for i in range(2)]
            sbf = [av(A0 + 39 * KB + i * 2 * KB, 2 * KB) for i in range(2)]
            Pb = [avb(A0 + 43 * KB + i * KB, 512) for i in range(4)]
            osw = [avb(A0 + 47 * KB + i * 512, 256) for i in range(2)]
            sml = av(A0 + 48 * KB, 256)

            if "swa" in phases or "mix" in phases:
                kq = wslot()
                wq_v = wpool[:, kq, :].rearrange("p (c n) -> p c n", c=8)
                R.dma("pool", wq_v, w_in[:, 1536:2048].rearrange("(c p) n -> p c n", p=128), [], [("w", kq, 0)],
                      key=("w", kq, 0))
                kk = wslot()
                wkv_v = wpool[:, kk, 0:2048].rearrange("p (c n) -> p c n", c=8)
                R.dma("pool", wkv_v, w_in[:, 2048:2304].rearrange("(c p) n -> p c n", p=128), [], [("w", kk, 0)],
                      key=("w", kk, 0))
                R.dma("sp", bia, biast_d.rearrange("(c p) h q -> p c h q", p=128), [], ["bia"], key="bia")
                R.add("pool", lambda e: e.affine_select(out=bia[:, 0, :, :], in_=bia[:, 0, :, :],
                                                        pattern=[[0, 8], [-1, 128]], compare_op=ALU.is_gt, fill=NEG,
                                                        base=0, channel_multiplier=1), ["bia"], ["bia"])
                R.add("pool", lambda e: e.affine_select(out=bia[:, 1, :, :], in_=bia[:, 1, :, :],
                                                        pattern=[[0, 8], [1, 128]], compare_op=ALU.is_ge, fill=NEG,
                                                        base=0, channel_multiplier=-1), ["bia"], ["bia"])
                R.add("pool", lambda e: e.memset(vsw[:, :, :, 64:65], 1.0), [], ["vsw1"])
                for t in range(NT):
                    b = t % 2
                    pb = 4 + b * 2
                    for c in range(8):
                        R.add("pe", lambda e, c=c, t=t, pb=pb: e.matmul(
                            ps[:, pb, :], lhsT=hT[:, c, t * 128:(t + 1) * 128], rhs=wq_v[:, c, :], start=(c == 0),
                            stop=(c == 7)), [("hT", t), ("w", kq, 0)], [("ps", pb)])
                    for c in range(8):
                        R.add("pe", lambda e, c=c, t=t, pb=pb: e.matmul(
                            ps[:, pb + 1, 0:256], lhsT=hT[:, c, t * 128:(t + 1) * 128], rhs=wkv_v[:, c, :],
                            start=(c == 0), stop=(c == 7)), [("hT", t), ("w", kk, 0)], [("ps", pb + 1)])
                    qk = ps[:, pb:pb + 2, :].rearrange("p a n -> p (a n)")[:, 0:640]
                    R.add("act", lambda e, qk=qk: e.activation(out=sq[:, :], in_=qk, func=AF.Square),
                          [("ps", pb), ("ps", pb + 1)], ["sq"])
                    R.add("dve", lambda e: e.tensor_reduce(out=sml[:, 0:10], in_=sq.rearrange("p (h d) -> p h d", d=64),
                                                           axis=AX.X, op=ALU.add), ["sq"], ["sml"])
                    R.add("act", lambda e: e.activation(out=sml[:, 16:26], in_=sml[:, 0:10], func=AF.Ln, bias=EPS,
                                                        scale=1.0 / 64), ["sml"], ["sml2"])
                    R.add("act", lambda e, b=b: e.activation(out=sml[:, 32 + b * 16:42 + b * 16], in_=sml[:, 16:26],
                                                             func=AF.Exp, scale=-0.5), ["sml2"], [("srs", b)])
                    R.add("dve", lambda e, b=b, qk=qk: e.tensor_tensor(
                        out=qn[b].rearrange("p (h d) -> p h d", d=64), in0=qk.rearrange("p (h d) -> p h d", d=64),
                        in1=sml[:, 32 + b * 16:42 + b * 16].unsqueeze(2).to_broadcast([128, 10, 64]), op=ALU.mult),
                        [("ps", pb), ("ps", pb + 1), ("srs", b)], [("qn", b)])
                    R.add("dve", lambda e, t=t, pb=pb: e.tensor_copy(
                        out=vsw[:, t, :, 0:64], in_=ps[:, pb + 1, 128:256].rearrange("p (g d) -> p g d", g=2)),
                        [("ps", pb + 1)], [("vsw", t)])
                    tp = ps[:, b, :].bitcast(BF16)
                    for c in range(5):
                        R.add("pe", lambda e, c=c, b=b, tp=tp: e.transpose(
                            out=tp[:, c * 128:(c + 1) * 128], in_=qn[b][:, c * 128:(c + 1) * 128],
                            identity=identb[:, :]), [("qn", b), "identb"], [("ps", b)])
                    R.add("dve", lambda e, t=t, tp=tp: e.tensor_scalar(
                        out=qTs[:, :, t * 128:(t + 1) * 128], in0=tp[:, 0:512].rearrange("p (i n) -> p i n", i=4),
                        scalar1=cst[:, ccol(l, C_SWQ):ccol(l, C_SWQ) + 1], scalar2=0.125, op0=ALU.mult,
                        op1=ALU.mult), [("ps", b)], [("qTs", t)])
                    R.add("dve", lambda e, t=t, tp=tp: e.tensor_scalar(
                        out=kTs[:, t * 128:(t + 1) * 128], in0=tp[:, 512:640],
                        scalar1=cst[:, ccol(l, C_SWK):ccol(l, C_SWK) + 1], scalar2=None, op0=ALU.mult),
                        [("ps", b)], [("kTs", t)])
                it = 0
                for n in range(NT):
                    for g in range(2):
                        ob = 6 + ((n * 2 + g) % 2)
                        chunks = [(1, n)] if n == 0 else [(0, n - 1), (1, n)]
                        slots = []
                        for ci, (cc, kt) in enumerate(chunks):
                            sbk = it % 4
                            it += 1
                            slots.append(sbk)
                            zb = 2 + (sbk % 2)
                            R.add("pe", lambda e, kt=kt, n=n, g=g, zb=zb: e.matmul(
                                ps[:, zb, :], lhsT=kTs[g * 64:(g + 1) * 64, kt * 128:(kt + 1) * 128],
                                rhs=qTs[g * 64:(g + 1) * 64, :, n * 128:(n + 1) * 128], start=True, stop=True),
                                [("kTs", kt), ("qTs", n)], [("ps", zb)])
                            R.add("dve", lambda e, zb=zb, sbk=sbk, cc=cc, g=g: e.tensor_tensor(
                                out=sbf[sbk % 2].rearrange("p (i q) -> p i q", i=4),
                                in0=ps[:, zb, :].rearrange("p (i q) -> p i q", i=4),
                                in1=bia[:, cc, g * 4:(g + 1) * 4, :], op=ALU.add), [("ps", zb), "bia"],
                                [("sbf", sbk % 2)])
                            R.add("act", lambda e, sbk=sbk: e.activation(out=Pb[sbk][:, :], in_=sbf[sbk % 2][:, :],
                                                                         func=AF.Exp), [("sbf", sbk % 2)], [("P", sbk)])
                        for i in range(4):
                            for ci, (cc, kt) in enumerate(chunks):
                                sbk = slots[ci]
                                R.add("pe", lambda e, i=i, sbk=sbk, kt=kt, g=g, ob=ob, ci=ci, nck=len(chunks): e.matmul(
                                    ps[:, ob, i * 65:(i + 1) * 65], lhsT=Pb[sbk][:, i * 128:(i + 1) * 128],
                                    rhs=vsw[:, kt, g, :], start=(ci == 0), stop=(ci == nck - 1)),
                                    [("P", sbk), ("vsw", kt), "vsw1"], [("ps", ob)])
                        ov = ps[:, ob, 0:260].rearrange("p (i d) -> p i d", i=4)
                        osb = (n * 2 + g) % 2
                        R.add("dve", lambda e, ov=ov, g=g: e.tensor_tensor(
                            out=sml[:, 0:4], in0=ov[:, :, 64],
                            in1=cst[:, C_ESINK + l * 8 + g * 4:C_ESINK + l * 8 + g * 4 + 4], op=ALU.add),
                            [("ps", ob), "esink"], ["den"])
                        R.add("dve", lambda e: e.reciprocal(out=sml[:, 4:8], in_=sml[:, 0:4]), ["den"], ["rec"])
                        R.add("dve", lambda e, ov=ov, osb=osb: e.tensor_tensor(
                            out=osw[osb].rearrange("p (i d) -> p i d", i=4), in0=ov[:, :, 0:64],
                            in1=sml[:, 4:8].unsqueeze(2).to_broadcast([128, 4, 64]), op=ALU.mult),
                            [("ps", ob), "rec"], [("osw", osb)])
                        R.dma("sp", mixed_d[n * 128:(n + 1) * 128, 512 + g * 256:512 + (g + 1) * 256], osw[osb][:, :],
                              [("osw", osb)], [("mixed", n)], key=("osw", osb))
            barrier()
            raw = [avb(6 * KB + i * 2 * KB, 1024) for i in range(2)]

            def raw_src(t):
                b = t % 2
                R.dma("sp", raw[b][:, :], mixed_d[t * 128:(t + 1) * 128, :], [("mixed", t)], [("raw", b)],
                      key=("raw", b))
                return raw[b]
            norm_T(raw_src, lambda t: ("raw", t % 2), NT, [(0, 512), (512, 1024)], ccol(l, C_GOUT),
                   lambda t: hT[:, :, t * 128:(t + 1) * 128], lambda t: ("hT", t))
            out_proj(wd_["w_out"][l], 8)
            barrier()

        def xattn(l, s):
            A0 = 6 * KB
            memt = [av(A0 + i * 4 * KB, 4 * KB) for i in range(2)]
            hmT = avb(A0 + 8 * KB, 2048).rearrange("p (c n) -> p c n", c=8)
            kTx = avb(A0 + 12 * KB, 1024).rearrange("p (h n) -> p h n", h=4)
            vx = avb(A0 + 14 * KB, 2 * 4 * 129 + 16)[:, 0:2 * 4 * 129].rearrange("p (m h d) -> p m h d", m=2, h=4)
            qTx = avb(A0 + 17 * KB, 8192).rearrange("p (h n) -> p h n", h=4)
            sq = av(A0 + 33 * KB, 2 * KB)
            qn = [avb(A0 + 35 * KB + i * KB, 512) for i in range(2)]
            Pb = [avb(A0 + 37 * KB + i * KB, 512) for i in range(4)]
            xor_ = [avb(A0 + 41 * KB + i * 4 * KB, 2048).rearrange("p (j n) -> p j n", j=4) for i in range(2)]
            sml = av(A0 + 49 * KB, 256)
            kwq = wslot()
            wq_v = wpool[:, kwq, :].rearrange("p (c n) -> p c n", c=8)
            R.dma("pool", wq_v, wd_["xattn_wq"][l].rearrange("(c p) n -> p c n", p=128), [], [("w", kwq, 0)],
                  key=("w", kwq, 0))
            kvs = []
            for half in range(2):
                k = wslot()
                wv = wpool[:, k, :].rearrange("p (c n) -> p c n", c=8)
                R.dma("pool", wv, wd_["xattn_wkv"][l][:, half * 512:(half + 1) * 512].rearrange(
                    "(c p) n -> p c n", p=128), [], [("w", k, 0)], key=("w", k, 0))
                kvs.append((k, wv))
            def mem_src(t):
                R.dma("sp", memt[t][:, :], mem_d[s, t * 128:(t + 1) * 128, :], [], [("memt", t)], key=("memt", t))
                return memt[t]
            norm_T(mem_src, lambda t: ("memt", t), 2, [(0, D)], ccol(l, C_GMEM),
                   lambda t: hmT[:, :, t * 128:(t + 1) * 128], lambda t: ("hmT", t))
            x_norm_T(ccol(l, C_GXAT))
            R.add("pool", lambda e: e.memset(vx[:, :, :, 128:129], 1.0), [], ["vx1"])

            def head_norm_T(pb, srcv, b, nh, gcolumn, scale, dst, dtok, stoks):
                R.add("act", lambda e: e.activation(out=sq[:, 0:nh * 128], in_=srcv, func=AF.Square), stoks, ["sq"])
                R.add("dve", lambda e: e.tensor_reduce(out=sml[:, 0:nh],
                                                       in_=sq[:, 0:nh * 128].rearrange("p (h d) -> p h d", d=128),
                                                       axis=AX.X, op=ALU.add), ["sq"], ["sml"])
                R.add("act", lambda e: e.activation(out=sml[:, 16:16 + nh], in_=sml[:, 0:nh], func=AF.Ln, bias=EPS,
                                                    scale=1.0 / 128), ["sml"], ["sml2"])
                R.add("act", lambda e: e.activation(out=sml[:, 32 + b * 8:32 + b * 8 + nh], in_=sml[:, 16:16 + nh],
                                                    func=AF.Exp, scale=-0.5), ["sml2"], [("srs", b)])
                R.add("dve", lambda e: e.tensor_tensor(
                    out=qn[b][:, 0:nh * 128].rearrange("p (h d) -> p h d", d=128),
                    in0=srcv.rearrange("p (h d) -> p h d", d=128),
                    in1=sml[:, 32 + b * 8:32 + b * 8 + nh].unsqueeze(2).to_broadcast([128, nh, 128]), op=ALU.mult),
                    stoks + [("srs", b)], [("qn", b)])
                tp = ps[:, b, :].bitcast(BF16)
                for c in range(nh):
                    R.add("pe", lambda e, c=c: e.transpose(out=tp[:, c * 128:(c + 1) * 128],
                                                           in_=qn[b][:, c * 128:(c + 1) * 128], identity=identb[:, :]),
                          [("qn", b), "identb"], [("ps", b)])
                R.add("dve", lambda e: e.tensor_scalar(
                    out=dst, in0=tp[:, 0:nh * 128].rearrange("p (h n) -> p h n", h=nh),
                    scalar1=cst[:, gcolumn:gcolumn + 1], scalar2=scale, op0=ALU.mult, op1=ALU.mult),
                    [("ps", b)], [dtok])

            for mt in range(2):
                pb = 4 + mt * 2
                for half in range(2):
                    k, wv = kvs[half]
                    for c in range(8):
                        R.add("pe", lambda e, c=c, wv=wv, half=half, mt=mt, pb=pb: e.matmul(
                            ps[:, pb + half, :], lhsT=hmT[:, c, mt * 128:(mt + 1) * 128], rhs=wv[:, c, :],
                            start=(c == 0), stop=(c == 7)), [("hmT", mt), ("w", k, 0)], [("ps", pb + half)])
                head_norm_T(pb, ps[:, pb, :], mt, 4, ccol(l, C_XK), 1.0, kTx[:, :, mt * 128:(mt + 1) * 128],
                            ("kTx", mt), [("ps", pb)])
                R.add("dve", lambda e, mt=mt, pb=pb: e.tensor_copy(
                    out=vx[:, mt, :, 0:128], in_=ps[:, pb + 1, :].rearrange("p (h d) -> p h d", h=4)),
                    [("ps", pb + 1)], [("vx", mt)])
            for t in range(NT):
                b = t % 2
                pb = 4 + b
                for c in range(8):
                    R.add("pe", lambda e, c=c, t=t, pb=pb: e.matmul(
                        ps[:, pb, :], lhsT=hT[:, c, t * 128:(t + 1) * 128], rhs=wq_v[:, c, :], start=(c == 0),
                        stop=(c == 7)), [("hT", t), ("w", kwq, 0)], [("ps", pb)])
                head_norm_T(pb, ps[:, pb, :], b, 4, ccol(l, C_XQ), 128 ** -0.5, qTx[:, :, t * 128:(t + 1) * 128],
                            ("qTx", t), [("ps", pb)])
            it = 0
            for qg in range(4):
                xb = qg % 2
                for h in range(4):
                    ob = 4 + (h % 2) * 2
                    slots = []
                    for kt in range(2):
                        sbk = it % 4
                        it += 1
                        slots.append(sbk)
                        zb = 2 + (sbk % 2)
                        R.add("pe", lambda e, h=h, kt=kt, qg=qg, zb=zb: e.matmul(
                            ps[:, zb, :], lhsT=kTx[:, h, kt * 128:(kt + 1) * 128],
                            rhs=qTx[:, h, qg * 512:(qg + 1) * 512], start=True, stop=True),
                            [("kTx", kt)] + [("qTx", qg * 4 + i) for i in range(4)], [("ps", zb)])
                        R.add("act", lambda e, zb=zb, sbk=sbk: e.activation(out=Pb[sbk][:, :], in_=ps[:, zb, :],
                                                                            func=AF.Exp), [("ps", zb)], [("P", sbk)])
                    for jj in range(4):
                        for kt in range(2):
                            sbk = slots[kt]
                            R.add("pe", lambda e, jj=jj, sbk=sbk, kt=kt, h=h, ob=ob: e.matmul(
                                ps[:, ob + jj // 2, (jj % 2) * 129:(jj % 2 + 1) * 129],
                                lhsT=Pb[sbk][:, jj * 128:(jj + 1) * 128], rhs=vx[:, kt, h, :], start=(kt == 0),
                                stop=(kt == 1)), [("P", sbk), ("vx", kt), "vx1"], [("ps", ob + jj // 2)])
                    ov = ps[:, ob:ob + 2, 0:258].rearrange("p a (j d) -> p a j d", j=2)
                    R.add("dve", lambda e, ov=ov: e.reciprocal(out=sml[:, 48:52].rearrange("p (a j) -> p a j", a=2),
                                                               in_=ov[:, :, :, 128]),
                          [("ps", ob), ("ps", ob + 1)], ["rec"])
                    for a2 in range(2):
                        R.add("dve", lambda e, ov=ov, a2=a2, h=h, xb=xb: e.tensor_tensor(
                            out=xor_[xb][:, a2 * 2:(a2 + 1) * 2, h * 128:(h + 1) * 128], in0=ov[:, a2, :, 0:128],
                            in1=sml[:, 48 + a2 * 2:50 + a2 * 2].unsqueeze(2).to_broadcast([128, 2, 128]), op=ALU.mult),
                            [("ps", ob + a2), "rec"], [("xor", xb)])
                for jj in range(4):
                    t = qg * 4 + jj
                    b = t % 2
                    tp = ps[:, b, :].bitcast(BF16)
                    for c in range(4):
                        R.add("pe", lambda e, c=c, jj=jj, xb=xb, tp=tp: e.transpose(
                            out=tp[:, c * 128:(c + 1) * 128], in_=xor_[xb][:, jj, c * 128:(c + 1) * 128],
                            identity=identb[:, :]), [("xor", xb), "identb"], [("ps", b)])
                    R.add("act", lambda e, t=t, tp=tp: e.activation(
                        out=hT[:, 0:4, t * 128:(t + 1) * 128], in_=tp[:, 0:512].rearrange("p (c n) -> p c n", c=4),
                        func=AF.Copy), [("ps", b)], [("hT", t)])
            out_proj(wd_["xattn_wo"][l], 4)
            barrier()

        def ffn(l):
            i = l // 2
            moe = (l % 2 == 1)
            A0 = 6 * KB
            actT = avb(A0, 8192).rearrange("p (j n) -> p j n", j=4)
            sg = [av(A0 + 16 * KB + k * 2 * KB, 2 * KB) for k in range(2)]
            hn32 = av(A0 + 20 * KB, 4 * KB)
            h32T = av(A0 + 24 * KB, 4 * KB).rearrange("p (c n) -> p c n", c=8)
            gates = av(A0 + 28 * KB, 512).rearrange("p (t e) -> p t e", t=16)
            sm = av(A0 + 29 * KB, 512)

            def router_tile(t, b, src, stok):
                R.add("dve", lambda e: e.tensor_scalar(out=hn32[:, :], in0=src, scalar1=stat[:, 16 + b * 4:17 + b * 4],
                                                       scalar2=None, op0=ALU.mult), [stok, ("rstd", b)], ["hn32"])
                tpf = ps[:, 2:4, :].rearrange("p a n -> p (a n)")
                for c in range(8):
                    R.add("pe", lambda e, c=c: e.transpose(out=tpf[:, c * 128:(c + 1) * 128],
                                                           in_=hn32[:, c * 128:(c + 1) * 128], identity=identf[:, :]),
                          ["hn32", "identf"], [("ps", 2), ("ps", 3)])
                gc = ccol(l, C_GFFN)
                R.add("dve", lambda e: e.tensor_tensor(
                    out=h32T, in0=tpf.rearrange("p (c n) -> p c n", c=8),
                    in1=cst[:, gc:gc + 8].unsqueeze(2).to_broadcast([128, 8, 128]), op=ALU.mult),
                    [("ps", 2), ("ps", 3)], ["h32T"])
                for c in range(8):
                    R.add("pe", lambda e, c=c: e.matmul(ps[:, 4, 0:NE], lhsT=h32T[:, c, :], rhs=rw[:, i, c, :],
                                                        start=(c == 0), stop=(c == 7)), ["h32T"], [("ps", 4)])
                lg, eq1, l2, eq2 = sm[:, 0:8], sm[:, 8:16], sm[:, 16:24], sm[:, 24:32]
                m1, m2, dd, ee, g1, g2 = (sm[:, 32:33], sm[:, 33:34], sm[:, 34:35], sm[:, 35:36], sm[:, 36:37],
                                          sm[:, 37:38])
                ga = sm[:, 40:48]
                dv = lambda fn, r, w: R.add("dve", fn, r, w)
                dv(lambda e: e.tensor_tensor(out=lg, in0=ps[:, 4, 0:NE], in1=cst[:, C_RB + i * 8:C_RB + i * 8 + 8],
                                             op=ALU.add), [("ps", 4)], ["r0"])
                dv(lambda e: e.tensor_reduce(out=m1, in_=lg, axis=AX.X, op=ALU.max), ["r0"], ["r1"])
                dv(lambda e: e.tensor_scalar(out=eq1, in0=lg, scalar1=m1, scalar2=None, op0=ALU.is_equal),
                   ["r0", "r1"], ["r2"])
                dv(lambda e: e.scalar_tensor_tensor(out=l2, in0=eq1, scalar=-1e30, in1=lg, op0=ALU.mult, op1=ALU.add),
                   ["r2", "r0"], ["r3"])
                dv(lambda e: e.tensor_reduce(out=m2, in_=l2, axis=AX.X, op=ALU.max), ["r3"], ["r4"])
                dv(lambda e: e.tensor_scalar(out=eq2, in0=l2, scalar1=m2, scalar2=None, op0=ALU.is_equal),
                   ["r3", "r4"], ["r5"])
                dv(lambda e: e.tensor_tensor(out=dd, in0=m2, in1=m1, op=ALU.subtract), ["r4", "r1"], ["r6"])
                R.add("act", lambda e: e.activation(out=ee, in_=dd, func=AF.Exp), ["r6"], ["r7"])
                dv(lambda e: e.tensor_scalar(out=g1, in0=ee, scalar1=1.0, scalar2=None, op0=ALU.add), ["r7"], ["r8"])
                dv(lambda e: e.reciprocal(out=g1, in_=g1), ["r8"], ["r8"])
                dv(lambda e: e.tensor_tensor(out=g2, in0=ee, in1=g1, op=ALU.mult), ["r7", "r8"], ["r9"])
                dv(lambda e: e.tensor_scalar(out=ga, in0=eq1, scalar1=g1, scalar2=None, op0=ALU.mult),
                   ["r2", "r8"], ["r10"])
                dv(lambda e: e.scalar_tensor_tensor(out=gates[:, t, :], in0=eq2, scalar=g2, in1=ga, op0=ALU.mult,
                                                    op1=ALU.add), ["r5", "r9", "r10"], ["gates"])

            x_norm_T(ccol(l, C_GFFN), router_tile if moe else None)

            def ffn_weights(ei):
                if moe:
                    return wd_["exp_w_gate"][i, ei], wd_["exp_w_up"][i, ei], wd_["exp_w_down"][i, ei]
                return wd_["dense_w_gate"][i], wd_["dense_w_up"][i], wd_["dense_w_down"][i]

            for ei in range(NE if moe else 1):
                wg_d, wu_d, wdn_d = ffn_weights(ei)
                for fg in range(7):
                    f0 = fg * 512
                    kg, ku, kd = wslot(), wslot(), wslot()
                    wgv = wpool[:, kg, :].rearrange("p (c n) -> p c n", c=8)
                    wuv = wpool[:, ku, :].rearrange("p (c n) -> p c n", c=8)
                    wdv = wpool[:, kd, :].rearrange("p (j n) -> p j n", j=4)
                    R.dma("pool", wgv, wg_d[:, f0:f0 + 512].rearrange("(c p) f -> p c f", p=128), [], [("w", kg, 0)],
                          key=("w", kg, 0))
                    R.dma("pool", wuv, wu_d[:, f0:f0 + 512].rearrange("(c p) f -> p c f", p=128), [], [("w", ku, 0)],
                          key=("w", ku, 0))
                    R.dma("pool", wdv, wdn_d[f0:f0 + 512, :].rearrange("(j p) d -> p j d", p=128), [], [("w", kd, 0)],
                          key=("w", kd, 0))
                    kk = 0
                    for tg in range(4):
                        hr = [("hT", tg * 4 + q) for q in range(4)]
                        for j in range(4):
                            pb = (kk % 2) * 2
                            sb_ = kk % 2
                            kk += 1
                            for c in range(8):
                                R.add("pe", lambda e, c=c, j=j, tg=tg, pb=pb, wgv=wgv: e.matmul(
                                    ps[:, pb, :], lhsT=wgv[:, c, j * 128:(j + 1) * 128],
                                    rhs=hT[:, c, tg * 512:(tg + 1) * 512], start=(c == 0), stop=(c == 7)),
                                    hr + [("w", kg, 0)], [("ps", pb)])
                            for c in range(8):
                                R.add("pe", lambda e, c=c, j=j, tg=tg, pb=pb, wuv=wuv: e.matmul(
                                    ps[:, pb + 1, :], lhsT=wuv[:, c, j * 128:(j + 1) * 128],
                                    rhs=hT[:, c, tg * 512:(tg + 1) * 512], start=(c == 0), stop=(c == 7)),
                                    hr + [("w", ku, 0)], [("ps", pb + 1)])
                            R.add("act", lambda e, pb=pb, sb_=sb_: e.activation(out=sg[sb_][:, :], in_=ps[:, pb, :],
                                                                                func=AF.Silu),
                                  [("ps", pb)], [("sg", sb_)])
                            R.add("dve", lambda e, pb=pb, sb_=sb_, j=j, tg=tg: e.tensor_tensor(
                                out=actT[:, j, tg * 512:(tg + 1) * 512], in0=sg[sb_][:, :], in1=ps[:, pb + 1, :],
                                op=ALU.mult), [("sg", sb_), ("ps", pb + 1)], [("actT", tg)])
                    for t in range(NT):
                        pb = 4 + (t % 2) * 2
                        for half in range(2):
                            for j in range(4):
                                R.add("pe", lambda e, t=t, j=j, half=half, pb=pb, wdv=wdv: e.matmul(
                                    ps[:, pb + half, :], lhsT=actT[:, j, t * 128:(t + 1) * 128],
                                    rhs=wdv[:, j, half * 512:(half + 1) * 512], start=(j == 0), stop=(j == 3)),
                                    [("actT", t // 4), ("w", kd, 0)], [("ps", pb + half)])
                        add_to_x(t, pb, gates[:, t, ei:ei + 1] if moe else None)
            barrier()

        for s in range(nseq):
            for t in range(NT):
                R.dma("sp", x[:, t, :], x_d[s, t * 128:(t + 1) * 128, :], [], [("x", t)], key=("x", t))
            for l in layers:
                if "mix" in phases or "sb" in phases or "swa" in phases:
                    mixer(l)
                if "xat" in phases:
                    xattn(l, s)
                if "ffn" in phases:
                    ffn(l)
            for t in range(NT):
                R.dma("sp", y_d[s, t * 128:(t + 1) * 128, :], x[:, t, :], [("x", t)], [("y", s, t)], key=("x", t))
        R.add("sp", None, [("y", s, t) for s in range(nseq) for t in range(NT)], [])
        nsem = R.emit(nc)
        build_program.info = (R.nops, nsem)
    return nc


def _t5_buckets(dist):
    n = np.maximum(dist, 0)
    max_exact = 16
    large = max_exact + (np.log(np.maximum(n, 1) / max_exact) / np.log(128 / max_exact) * (32 - max_exact)).astype(
        np.int32)
    large = np.minimum(large, 31)
    return np.where(n < max_exact, n, large).astype(np.int32)


def prep_shared(inputs):
    rel_bias = np.asarray(inputs["rel_bias"], dtype=np.float32)
    dist = 128 + np.arange(128)[:, None] - np.arange(256)[None, :]
    bt = rel_bias[_t5_buckets(dist)]
    biast = np.ascontiguousarray(np.transpose(bt, (1, 2, 0)))
    w_in = np.array(inputs["w_in"], dtype=np.float32, copy=True)
    q = w_in[:, :, 1536:2048].reshape(DEPTH, D, 2, 4, 64)
    w_in[:, :, 1536:2048] = np.transpose(q, (0, 1, 3, 2, 4)).reshape(DEPTH, D, 512)
    shared = {n: np.ascontiguousarray(np.asarray(inputs[n], dtype=np.float32)) for n in W_NAMES if n != "w_in"}
    shared["w_in"] = w_in
    shared["swa_bias_t"] = biast
    return shared


_CACHE = {}


def kernel(**inputs):
    n_cores = 8
    x = np.asarray(inputs["x"], dtype=np.float32)
    mem = np.asarray(inputs["mem"], dtype=np.float32)
    B = x.shape[0]
    nseq = B // n_cores
    if "nc" not in _CACHE:
        _CACHE["nc"] = build_program(nseq=nseq)
    nc = _CACHE["nc"]
    shared = prep_shared(inputs)
    in_maps = []
    for c in range(n_cores):
        m = dict(shared)
        m["x"] = np.ascontiguousarray(x[c * nseq:(c + 1) * nseq])
        m["mem"] = np.ascontiguousarray(mem[c * nseq:(c + 1) * nseq])
        in_maps.append(m)
    res = run_bass_kernel_spmd(nc, in_maps, core_ids=list(range(n_cores)))
    return np.concatenate([r["y"] for r in res.results], axis=0)
```

```python
import contextlib
import numpy as np
import concourse.bass as bass
import concourse.mybir as mybir
from concourse.bass_utils import run_bass_kernel_spmd

F32 = mybir.dt.float32
BF16 = mybir.dt.bfloat16
AF = mybir.ActivationFunctionType
ALU = mybir.AluOpType
AX = mybir.AxisListType

S = 2048
D = 1024
NT = 16
DEPTH = 4
DFF = 3584
NE = 8
NM = 256
EPS = 1e-6
NEG = -30000.0


class Op:
    __slots__ = ("eng", "fn", "deps", "dma_key", "idx", "inc", "val")


class Rec:
    ENGS = ("pe", "act", "dve", "pool", "sp")

    def __init__(self):
        self.ops = {e: [] for e in self.ENGS}
        self.lastw = {}
        self.readers = {}
        self.dma_cnt = {}
        self.last_dma = {}
        self.floor = None
        self.nops = 0

    @staticmethod
    def _key(d):
        if d.dma_key is not None:
            return ("dma", d.dma_key)
        return ("eng", d.eng)

    def add(self, eng, fn, reads=(), writes=(), dma_key=None, extra=()):
        op = Op()
        op.eng = eng
        op.fn = fn
        op.dma_key = dma_key
        op.inc = dma_key is not None
        deps = {}

        def adddep(d):
            if d.dma_key is None and d.eng == "pe" and eng == "pe" and dma_key is None:
                return
            k = self._key(d)
            v = d.val if d.dma_key is not None else d.idx
            cur = deps.get(k)
            if cur is None or cur[0] < v:
                deps[k] = (v, d)

        if self.floor is not None:
            adddep(self.floor)
        for d in extra:
            adddep(d)
        for t in reads:
            d = self.lastw.get(t)
            if d is not None:
                adddep(d)
        for t in writes:
            d = self.lastw.get(t)
            if d is not None:
                adddep(d)
            rs = self.readers.get(t)
            if rs:
                for (_, r) in rs.values():
                    adddep(r)
        lst = self.ops[eng]
        op.idx = len(lst)
        if dma_key is not None:
            c = self.dma_cnt.get(dma_key, 0) + 16
            self.dma_cnt[dma_key] = c
            op.val = c
            self.last_dma[dma_key] = op
        else:
            op.val = None
        op.deps = [d for (_, d) in deps.values()]
        for d in op.deps:
            d.inc = True
        lst.append(op)
        self.nops += 1
        for t in writes:
            self.lastw[t] = op
            self.readers[t] = {}
        k = self._key(op)
        v = op.val if dma_key is not None else op.idx
        for t in reads:
            rs = self.readers.get(t)
            if rs is None:
                rs = self.readers[t] = {}
            rs[k] = (v, op)
        return op

    def barrier(self, fn):
        extra = []
        for e in ("pe", "act", "dve", "pool"):
            for op in reversed(self.ops[e]):
                if op.dma_key is None and op.fn is not None:
                    extra.append(op)
                    break
        extra.extend(self.last_dma.values())
        b = self.add("pool", fn, (), (), extra=extra)
        self.floor = b
        return b

    def dma(self, eng, out, in_, reads, writes, key, **kw):
        return self.add(eng, lambda e: e.dma_start(out=out, in_=in_, **kw), reads, writes, dma_key=key)

    def emit(self, nc):
        for e in self.ENGS:
            c = 0
            for op in self.ops[e]:
                if op.dma_key is None:
                    if op.inc:
                        c += 1
                    op.val = c
        with contextlib.ExitStack() as st:
            sems = {}
            for e in ("pe", "act", "dve", "pool"):
                sems[("eng", e)] = st.enter_context(nc.semaphore("s_" + e))
            for i, k in enumerate(self.dma_cnt.keys()):
                sems[("dma", k)] = st.enter_context(nc.semaphore("d%d" % i))
            block = st.enter_context(nc.Block())

            def replay(ename):
                def body(eng):
                    waited = {}
                    for op in self.ops[ename]:
                        for d in op.deps:
                            k = self._key(d)
                            if waited.get(k, 0) < d.val:
                                eng.wait_ge(sems[k], d.val)
                                waited[k] = d.val
                        if op.fn is None:
                            continue
                        ins = op.fn(eng)
                        if op.dma_key is not None:
                            ins.then_inc(sems[("dma", op.dma_key)], 16)
                        elif op.inc:
                            ins.then_inc(sems[("eng", ename)], 1)
                return body

            block.tensor(replay("pe"))
            block.scalar(replay("act"))
            block.vector(replay("dve"))
            block.gpsimd(replay("pool"))
            block.sync(replay("sp"))
        return len(self.dma_cnt)


W_NAMES = ["norm_mix", "w_in", "sb_out_gain", "swa_q_gain", "swa_k_gain", "swa_sinks", "swa_out_gain",
           "w_out", "norm_xattn", "norm_mem", "xattn_wq", "xattn_wkv", "xattn_q_gain", "xattn_k_gain",
           "xattn_wo", "norm_ffn", "dense_w_gate", "dense_w_up", "dense_w_down", "router_w", "router_b",
           "exp_w_gate", "exp_w_up", "exp_w_down"]
W_SHAPES = {
    "norm_mix": [DEPTH, D], "w_in": [DEPTH, D, 2304], "sb_out_gain": [DEPTH, 512], "swa_q_gain": [DEPTH, 64],
    "swa_k_gain": [DEPTH, 64], "swa_sinks": [DEPTH, 8], "swa_out_gain": [DEPTH, 512], "w_out": [DEPTH, D, D],
    "norm_xattn": [DEPTH, D], "norm_mem": [DEPTH, D], "xattn_wq": [DEPTH, D, 512], "xattn_wkv": [DEPTH, D, 1024],
    "xattn_q_gain": [DEPTH, 128], "xattn_k_gain": [DEPTH, 128], "xattn_wo": [DEPTH, 512, D], "norm_ffn": [DEPTH, D],
    "dense_w_gate": [2, D, DFF], "dense_w_up": [2, D, DFF], "dense_w_down": [2, DFF, D], "router_w": [2, D, NE],
    "router_b": [2, NE], "exp_w_gate": [2, NE, D, DFF], "exp_w_up": [2, NE, D, DFF], "exp_w_down": [2, NE, DFF, D],
}


def build_program(nseq=4, layers=(0, 1, 2, 3), phases=("mix", "xat", "ffn")):
    nc = bass.Bass("TRN2", target_bir_lowering=False)
    x_d = nc.dram_tensor("x", [nseq, S, D], F32, kind="ExternalInput").ap()
    mem_d = nc.dram_tensor("mem", [nseq, NM, D], F32, kind="ExternalInput").ap()
    biast_d = nc.dram_tensor("swa_bias_t", [256, 8, 128], F32, kind="ExternalInput").ap()
    wd_ = {n: nc.dram_tensor(n, W_SHAPES[n], F32, kind="ExternalInput").ap() for n in W_NAMES}
    y_d = nc.dram_tensor("y", [nseq, S, D], F32, kind="ExternalOutput").ap()
    mixed_d = nc.dram_tensor("mixed_scr", [S, D], BF16).ap()
    R = Rec()

    with contextlib.ExitStack() as st:
        x = st.enter_context(nc.sbuf_tensor("sb_x", [128, NT, D], F32))
        hT = st.enter_context(nc.sbuf_tensor("sb_hT", [128, 8, S], BF16))
        wpool = st.enter_context(nc.sbuf_tensor("sb_w", [128, 6, 4096], BF16))
        arena = st.enter_context(nc.sbuf_tensor("sb_arena", [128, 14336], F32))
        cst = st.enter_context(nc.sbuf_tensor("sb_cst", [128, 320], F32))
        rw = st.enter_context(nc.sbuf_tensor("sb_rw", [128, 2, 8, NE], F32))
        identf = st.enter_context(nc.sbuf_tensor("sb_identf", [128, 128], F32))
        identb = st.enter_context(nc.sbuf_tensor("sb_identb", [128, 128], BF16))
        negtri = st.enter_context(nc.sbuf_tensor("sb_negtri", [128, 128], BF16))
        onesb = st.enter_context(nc.sbuf_tensor("sb_ones", [128, 128], BF16))
        maskd = st.enter_context(nc.sbuf_tensor("sb_maskd", [128, 128], BF16))
        tmpf = st.enter_context(nc.sbuf_tensor("sb_tmpf", [128, 128], F32))
        dummy = st.enter_context(nc.sbuf_tensor("sb_dummy", [128, 2], F32))
        stat = st.enter_context(nc.sbuf_tensor("sb_stat", [128, 64], F32))
        ps = st.enter_context(nc.psum_tensor("ps", [128, 8, 512], F32))

        def av(off, nbytes):
            assert off % 4 == 0 and nbytes % 4 == 0 and off + nbytes <= 57344, (off, nbytes)
            return arena[:, off // 4:(off + nbytes) // 4]

        def avb(off, nelem):
            return av(off, nelem * 2).bitcast(BF16)

        KB = 1024
        hn = [avb(0, 1024), avb(2 * KB, 1024)]
        junk = avb(4 * KB, 1024)

        def ccol(l, k):
            return l * 48 + k
        C_GMIX, C_GXAT, C_GFFN, C_GOUT, C_GMEM, C_SWQ, C_SWK, C_XQ, C_XK = 0, 8, 16, 24, 32, 40, 41, 42, 43
        C_ESINK = 192
        C_RB = 224

        bar_n = [0]

        def barrier():
            bar_n[0] += 1
            R.barrier(lambda e: e.memset(dummy[:, 0:1], 0.0))

        def cdma(out, in_, tok, **kw):
            R.dma("sp", out, in_, [], [tok], key=("c", tok), **kw)

        cidx = [0]

        def cload(out, in_, **kw):
            cidx[0] += 1
            R.dma("sp", out, in_, [], [("cst", cidx[0])], key=("c", cidx[0] % 4), **kw)

        def gT_load(col, vec):
            n = vec.shape[0] // 128
            cload(cst[:, col:col + n], vec.rearrange("(c p) -> p c", p=128), allow_slow_non_contiguous=True)

        for l in range(DEPTH):
            gT_load(ccol(l, C_GMIX), wd_["norm_mix"][l])
            gT_load(ccol(l, C_GXAT), wd_["norm_xattn"][l])
            gT_load(ccol(l, C_GFFN), wd_["norm_ffn"][l])
            gT_load(ccol(l, C_GOUT), wd_["sb_out_gain"][l])
            gT_load(ccol(l, C_GOUT) + 4, wd_["swa_out_gain"][l])
            gT_load(ccol(l, C_GMEM), wd_["norm_mem"][l])
            gT_load(ccol(l, C_XQ), wd_["xattn_q_gain"][l])
            gT_load(ccol(l, C_XK), wd_["xattn_k_gain"][l])
            for half in range(2):
                cload(cst[half * 64:(half + 1) * 64, ccol(l, C_SWQ):ccol(l, C_SWQ) + 1],
                      wd_["swa_q_gain"][l].rearrange("(d o) -> d o", o=1), allow_slow_non_contiguous=True)
                cload(cst[half * 64:(half + 1) * 64, ccol(l, C_SWK):ccol(l, C_SWK) + 1],
                      wd_["swa_k_gain"][l].rearrange("(d o) -> d o", o=1), allow_slow_non_contiguous=True)
        cload(cst[:, C_ESINK:C_ESINK + 32], wd_["swa_sinks"].rearrange("l h -> (l h)").partition_broadcast(128))
        cload(cst[:, C_RB:C_RB + 16], wd_["router_b"].rearrange("i e -> (i e)").partition_broadcast(128))
        for i in range(2):
            cload(rw[:, i, :, :], wd_["router_w"][i].rearrange("(c p) e -> p c e", p=128),
                  allow_slow_non_contiguous=True)
        R.add("pool", lambda e: e.memset(tmpf[:, :], 1.0), [], ["tmpf"])
        R.add("pool", lambda e: e.affine_select(out=identf[:, :], in_=tmpf[:, :], pattern=[[-1, 128]],
                                                compare_op=ALU.is_equal, fill=0.0, base=0, channel_multiplier=1),
              ["tmpf"], ["identf"])
        R.add("pool", lambda e: e.tensor_copy(out=identb[:, :], in_=identf[:, :]), ["identf"], ["identb"])
        R.add("pool", lambda e: e.tensor_copy(out=onesb[:, :], in_=tmpf[:, :]), ["tmpf"], ["onesb"])
        R.add("pool", lambda e: e.affine_select(out=maskd[:, :], in_=tmpf[:, :], pattern=[[1, 128]],
                                                compare_op=ALU.is_gt, fill=0.0, base=0, channel_multiplier=-1),
              ["tmpf"], ["maskd"])
        R.add("pool", lambda e: e.memset(tmpf[:, :], -1.0), ["tmpf"], ["tmpf"])
        R.add("pool", lambda e: e.affine_select(out=negtri[:, :], in_=tmpf[:, :], pattern=[[-1, 128]],
                                                compare_op=ALU.is_ge, fill=0.0, base=0, channel_multiplier=1),
              ["tmpf"], ["negtri"])
        barrier()
        R.add("act", lambda e: e.activation(out=cst[:, C_ESINK:C_ESINK + 32], in_=cst[:, C_ESINK:C_ESINK + 32],
                                            func=AF.Exp), [], ["esink"])
        barrier()

        wctr = [0]

        def wslot():
            k = wctr[0] % 6
            wctr[0] += 1
            return k

        def wload(k, part, col0, ncols, src):
            raise NotImplementedError

        def norm_T(src_fn, src_tok_fn, ntiles, groups, gcol, dst_fn, dst_tok_fn, extra_tile_fn=None):
            ng = len(groups)
            W = groups[-1][1]
            nch = W // 128
            for t in range(ntiles):
                b = t % 2
                src = src_fn(t)
                stok = src_tok_fn(t)
                ssv = stat[:, 0:ng]
                for gi, (c0, c1) in enumerate(groups):
                    R.add("act", lambda e, src=src, c0=c0, c1=c1, gi=gi: e.activation(
                        out=junk[:, c0:c1], in_=src[:, c0:c1], func=AF.Square, accum_out=stat[:, gi:gi + 1]),
                        [stok], ["junk", "stat"])
                wdt = groups[0][1] - groups[0][0]
                R.add("act", lambda e, ssv=ssv: e.activation(out=stat[:, 8:8 + ng], in_=ssv, func=AF.Ln, bias=EPS,
                                                            scale=1.0 / wdt), ["stat"], ["stat2"])
                R.add("act", lambda e, b=b: e.activation(out=stat[:, 16 + b * 4:16 + b * 4 + ng],
                                                         in_=stat[:, 8:8 + ng], func=AF.Exp, scale=-0.5),
                      ["stat2"], [("rstd", b)])
                for gi, (c0, c1) in enumerate(groups):
                    R.add("dve", lambda e, src=src, c0=c0, c1=c1, gi=gi, b=b: e.tensor_scalar(
                        out=hn[b][:, c0:c1], in0=src[:, c0:c1], scalar1=stat[:, 16 + b * 4 + gi:17 + b * 4 + gi],
                        scalar2=None, op0=ALU.mult), [stok, ("rstd", b)], [("hn", b)])
                if extra_tile_fn is not None:
                    extra_tile_fn(t, b, src, stok)
                tp = ps[:, b, :].bitcast(BF16)
                for c in range(nch):
                    R.add("pe", lambda e, c=c, b=b, tp=tp: e.transpose(
                        out=tp[:, c * 128:(c + 1) * 128], in_=hn[b][:, c * 128:(c + 1) * 128], identity=identb[:, :]),
                        [("hn", b), "identb"], [("ps", b)])
                dst = dst_fn(t)
                R.add("dve", lambda e, tp=tp, dst=dst: e.tensor_tensor(
                    out=dst, in0=tp[:, 0:nch * 128].rearrange("p (c n) -> p c n", c=nch),
                    in1=cst[:, gcol:gcol + nch].unsqueeze(2).to_broadcast([128, nch, 128]), op=ALU.mult),
                    [("ps", b)], [dst_tok_fn(t)])

        def x_norm_T(gcol, extra_tile_fn=None):
            norm_T(lambda t: x[:, t, :], lambda t: ("x", t), NT, [(0, D)], gcol,
                   lambda t: hT[:, :, t * 128:(t + 1) * 128], lambda t: ("hT", t), extra_tile_fn)

        def add_to_x(t, pb, gate_ap=None):
            src = ps[:, pb:pb + 2, :].rearrange("p a n -> p (a n)")
            if gate_ap is None:
                R.add("dve", lambda e: e.tensor_tensor(out=x[:, t, :], in0=src, in1=x[:, t, :], op=ALU.add),
                      [("ps", pb), ("ps", pb + 1), ("x", t)], [("x", t)])
            else:
                R.add("dve", lambda e: e.scalar_tensor_tensor(out=x[:, t, :], in0=src, scalar=gate_ap, in1=x[:, t, :],
                                                              op0=ALU.mult, op1=ALU.add),
                      [("ps", pb), ("ps", pb + 1), ("x", t), "gates"], [("x", t)])

        def out_proj(wsrc, nch):
            ks = []
            for half in range(2):
                k = wslot()
                wv = wpool[:, k, 0:nch * 512].rearrange("p (c n) -> p c n", c=nch)
                R.dma("pool", wv, wsrc[:, half * 512:(half + 1) * 512].rearrange("(c p) n -> p c n", p=128),
                      [], [("w", k, 0)], key=("w", k, 0))
                ks.append((k, wv))
            for t in range(NT):
                pb = 4 + (t % 2) * 2
                for half in range(2):
                    k, wv = ks[half]
                    for c in range(nch):
                        R.add("pe", lambda e, c=c, half=half, wv=wv, pb=pb, t=t: e.matmul(
                            ps[:, pb + half, :], lhsT=hT[:, c, t * 128:(t + 1) * 128], rhs=wv[:, c, :],
                            start=(c == 0), stop=(c == nch - 1)), [("hT", t), ("w", k, 0)], [("ps", pb + half)])
                add_to_x(t, pb)

        def mixer(l):
            w_in = wd_["w_in"][l]
            x_norm_T(ccol(l, C_GMIX))
            PB = 6 * KB

            def pair_bufs(i):
                o = PB + i * 12 * KB
                return (avb(o, 2048), avb(o + 4 * KB, 2048),
                        avb(o + 8 * KB, 2048).rearrange("p (t d) -> p t d", t=16))
            SO = PB + 24 * KB
            ebuf = [av(SO + i * 2 * KB, 2 * KB) for i in range(2)]
            spb = [avb(SO + 4 * KB + i * KB, 512) for i in range(2)]
            lgA = [av(SO + 6 * KB + i * 2 * KB, 2 * KB) for i in range(2)]
            Ab = [avb(SO + 10 * KB + i * KB, 512) for i in range(2)]
            Rb = [av(SO + 12 * KB + i * 2 * KB, 2 * KB) for i in range(2)]
            ost = [avb(SO + 16 * KB + i * KB, 512).rearrange("p (t d) -> p t d", t=4) for i in range(2)]

            def sb_proj(p):
                pi = p % 2
                qT, kT, vv = pair_bufs(pi)
                k = wslot()
                wv = wpool[:, k, 0:8 * 384].rearrange("p (c n) -> p c n", c=8)
                for part in range(3):
                    c0 = part * 512 + p * 128
                    R.dma("pool", wv[:, :, part * 128:(part + 1) * 128],
                          w_in[:, c0:c0 + 128].rearrange("(c p) n -> p c n", p=128), [], [("w", k, part)],
                          key=("w", k, part))
                gi = 0
                for part, dst in ((0, qT), (1, kT)):
                    for tg in range(4):
                        bk = 7 if gi % 2 == 0 else 4
                        gi += 1
                        for c in range(8):
                            R.add("pe", lambda e, c=c, tg=tg, part=part, wv=wv, bk=bk: e.matmul(
                                ps[:, bk, :], lhsT=wv[:, c, part * 128:(part + 1) * 128],
                                rhs=hT[:, c, tg * 512:(tg + 1) * 512], start=(c == 0), stop=(c == 7)),
                                [("hT", tg * 4 + i) for i in range(4)] + [("w", k, part)], [("ps", bk)])
                        sc = 0.125 if part == 0 else 1.0
                        R.add("act", lambda e, dst=dst, tg=tg, sc=sc, bk=bk: e.activation(
                            out=dst[:, tg * 512:(tg + 1) * 512], in_=ps[:, bk, :], func=AF.Copy, scale=sc),
                            [("ps", bk)], [("pq", pi, part, tg)])
                for t4 in range(4):
                    bk = 7 if gi % 2 == 0 else 4
                    gi += 1
                    for tt in range(4):
                        t = t4 * 4 + tt
                        for c in range(8):
                            R.add("pe", lambda e, c=c, t=t, tt=tt, wv=wv, bk=bk: e.matmul(
                                ps[:, bk, tt * 128:(tt + 1) * 128], lhsT=hT[:, c, t * 128:(t + 1) * 128],
                                rhs=wv[:, c, 256:384], start=(c == 0), stop=(c == 7)),
                                [("hT", t), ("w", k, 2)], [("ps", bk)])
                    R.add("dve", lambda e, vv=vv, t4=t4, bk=bk: e.tensor_copy(
                        out=vv[:, t4 * 4:(t4 + 1) * 4, :], in_=ps[:, bk, :].rearrange("p (t d) -> p t d", t=4)),
                        [("ps", bk)], [("pv", pi, t4)])

            def sb_attn(p):
                pi = p % 2
                qT, kT, vv = pair_bufs(pi)
                items = []
                for g in range(4):
                    for hp in range(2):
                        for a in range(4 * g + 3, -1, -1):
                            items.append((g, hp, a))
                ucount = [0]

                def unit_of(g, hp):
                    return g * 2 + hp

                def cols(g, a):
                    j = max(a - 4 * g, 0)
                    return j * 128, 512 - j * 128

                def st1(it, k):
                    g, hp, a = it
                    c0, n = cols(g, a)
                    pb = hp * 64
                    zb = k % 2
                    R.add("pe", lambda e: e.matmul(ps[:, zb, 0:n], lhsT=kT[pb:pb + 64, a * 128:(a + 1) * 128],
                                                   rhs=qT[pb:pb + 64, g * 512 + c0:(g + 1) * 512], start=True,
                                                   stop=True),
                          [("pq", pi, 0, g), ("pq", pi, 1, a // 4)], [("ps", zb)])
                    R.add("act", lambda e: e.activation(out=ebuf[zb][:, 0:n], in_=ps[:, zb, 0:n], func=AF.Exp),
                          [("ps", zb)], [("e", zb)])

                def st1b(it, k):
                    g, hp, a = it
                    c0, n = cols(g, a)
                    zb = k % 2
                    R.add("act", lambda e: e.activation(out=spb[zb][:, 0:n], in_=ebuf[zb][:, 0:n], func=AF.Ln,
                                                        bias=1.0), [("e", zb)], [("sp", zb)])
                    if a >= 4 * g:
                        R.add("pool", lambda e: e.tensor_tensor(out=spb[zb][:, 0:128], in0=spb[zb][:, 0:128],
                                                                in1=maskd[:, :], op=ALU.mult),
                              [("sp", zb), "maskd"], [("sp", zb)])

                def st2a(it, k):
                    g, hp, a = it
                    c0, n = cols(g, a)
                    pb = hp * 64
                    zb = k % 2
                    u = unit_of(g, hp) % 2
                    if a == 4 * g + 3:
                        R.add("pool", lambda e: e.memset(Rb[u][:, :], 0.0), [], [("R", u)])
                        R.add("dve", lambda e: e.memset(ps[:, 5 + u, 0:256], 0.0), [], [("ps", 5 + u)])
                    R.add("pe", lambda e: e.matmul(ps[:, 2 + zb, 0:n], lhsT=kT[pb:pb + 64, a * 128:(a + 1) * 128],
                                                   rhs=qT[pb:pb + 64, g * 512 + c0:(g + 1) * 512], start=True,
                                                   stop=False),
                          [("pq", pi, 0, g), ("pq", pi, 1, a // 4)], [("ps", 2 + zb)])
                    R.add("pe", lambda e: e.matmul(ps[:, 2 + zb, 0:n], lhsT=negtri[:, :], rhs=spb[zb][:, 0:n],
                                                   start=False, stop=True), [("sp", zb), "negtri"], [("ps", 2 + zb)])
                    R.add("pe", lambda e: e.matmul(ps[:, 4, 0:n], lhsT=onesb[:, :], rhs=spb[zb][:, 0:n], start=True,
                                                   stop=True), [("sp", zb), "onesb"], [("ps", 4)])
                    R.add("dve", lambda e: e.tensor_tensor(out=lgA[zb][:, 0:n], in0=ps[:, 2 + zb, 0:n],
                                                           in1=Rb[u][:, c0:512], op=ALU.subtract),
                          [("ps", 2 + zb), ("R", u)], [("lgA", zb)])
                    R.add("dve", lambda e: e.tensor_tensor(out=Rb[u][:, c0:512], in0=ps[:, 4, 0:n],
                                                           in1=Rb[u][:, c0:512], op=ALU.add),
                          [("ps", 4), ("R", u)], [("R", u)])

                def st2b(it, k):
                    g, hp, a = it
                    c0, n = cols(g, a)
                    zb = k % 2
                    R.add("act", lambda e: e.activation(out=Ab[zb][:, 0:n], in_=lgA[zb][:, 0:n], func=AF.Exp),
                          [("lgA", zb)], [("A", zb)])
                    if a >= 4 * g:
                        R.add("pool", lambda e: e.tensor_tensor(out=Ab[zb][:, 0:128], in0=Ab[zb][:, 0:128],
                                                                in1=maskd[:, :], op=ALU.mult),
                              [("A", zb), "maskd"], [("A", zb)])

                def st3(it, k):
                    g, hp, a = it
                    c0, n = cols(g, a)
                    zb = k % 2
                    u = unit_of(g, hp) % 2
                    ob = 5 + u
                    j0 = c0 // 128
                    for jj in range(j0, 4):
                        tile = 4 * g + jj
                        R.add("pe", lambda e, jj=jj, tile=tile: e.matmul(
                            ps[:, ob, jj * 64:(jj + 1) * 64], lhsT=Ab[zb][:, (jj - j0) * 128:(jj - j0 + 1) * 128],
                            rhs=vv[:, a, hp * 64:(hp + 1) * 64], start=False, stop=(a == 0),
                            skip_group_check=True),
                            [("A", zb), ("pv", pi, a // 4)], [("ps", ob)])
                    if a == 0:
                        osb = g % 2
                        R.add("dve", lambda e: e.tensor_copy(
                            out=ost[osb][:, :, hp * 64:(hp + 1) * 64],
                            in_=ps[:, ob, 0:256].rearrange("p (t d) -> p t d", t=4)),
                            [("ps", ob)], [("ost", osb)])
                        if hp == 1:
                            R.dma("sp", mixed_d[g * 512:(g + 1) * 512, p * 128:(p + 1) * 128].rearrange(
                                "(t q) d -> q t d", q=128), ost[osb][:, :, :], [("ost", osb)],
                                [("mixed", g * 4 + i) for i in range(4)], key=("ost", osb))

                n_it = len(items)
                for k in range(n_it + 2):
                    if k < n_it:
                        st1(items[k], k)
                    if 2 <= k:
                        st2b(items[k - 2], k - 2)
                    if k < n_it:
                        st1b(items[k], k)
                    if 1 <= k <= n_it:
                        st2a(items[k - 1], k - 1)
                    if 2 <= k:
                        st3(items[k - 2], k - 2)

            if "sb" in phases or "mix" in phases:
                sb_proj(0)
                for p in range(4):
                    if p + 1 < 4:
                        sb_proj(p + 1)
                    sb_attn(p)
            barrier()
            A0 = 6 * KB
            qTs = avb(A0, 8192).rearrange("p (i n) -> p i n", i=4)
            kTs = avb(A0 + 16 * KB, 2048)
            vsw = avb(A0 + 20 * KB, 16 * 2 * 65 + 32)[:, 0:16 * 2 * 65].rearrange("p (t g d) -> p t g d", t=16, g=2)
            bia = av(A0 + 25 * KB, 8 * KB).rearrange("p (c h q) -> p c h q", c=2, h=8)
            sq = av(A0 + 33 * KB, 640 * 4)
            qn = [avb(A0 + 36 * KB + i * 1536, 640) for i in range(2)]
            sbf = [av(A0 + 39 * KB + i * 2 * KB, 2 * KB) for i in range(2)]
            Pb = [avb(A0 + 43 * KB + i * KB, 512) for i in range(4)]
            osw = [avb(A0 + 47 * KB + i * 512, 256) for i in range(2)]
            sml = av(A0 + 48 * KB, 256)

            if "swa" in phases or "mix" in phases:
                kq = wslot()
                wq_v = wpool[:, kq, :].rearrange("p (c n) -> p c n", c=8)
                R.dma("pool", wq_v, w_in[:, 1536:2048].rearrange("(c p) n -> p c n", p=128), [], [("w", kq, 0)],
                      key=("w", kq, 0))
                kk = wslot()
                wkv_v = wpool[:, kk, 0:2048].rearrange("p (c n) -> p c n", c=8)
                R.dma("pool", wkv_v, w_in[:, 2048:2304].rearrange("(c p) n -> p c n", p=128), [], [("w", kk, 0)],
                      key=("w", kk, 0))
                R.dma("sp", bia, biast_d.rearrange("(c p) h q -> p c h q", p=128), [], ["bia"], key="bia")
                R.add("pool", lambda e: e.affine_select(out=bia[:, 0, :, :], in_=bia[:, 0, :, :],
                                                        pattern=[[0, 8], [-1, 128]], compare_op=ALU.is_gt, fill=NEG,
                                                        base=0, channel_multiplier=1), ["bia"], ["bia"])
                R.add("pool", lambda e: e.affine_select(out=bia[:, 1, :, :], in_=bia[:, 1, :, :],
                                                        pattern=[[0, 8], [1, 128]], compare_op=ALU.is_ge, fill=NEG,
                                                        base=0, channel_multiplier=-1), ["bia"], ["bia"])
                R.add("pool", lambda e: e.memset(vsw[:, :, :, 64:65], 1.0), [], ["vsw1"])
                swa_pend = [None]
                for t in range(NT):
                    b = t % 2
                    pb = 4 + b * 2
                    for c in range(8):
                        R.add("pe", lambda e, c=c, t=t, pb=pb: e.matmul(
                            ps[:, pb, :], lhsT=hT[:, c, t * 128:(t + 1) * 128], rhs=wq_v[:, c, :], start=(c == 0),
                            stop=(c == 7)), [("hT", t), ("w", kq, 0)], [("ps", pb)])
                    for c in range(8):
                        R.add("pe", lambda e, c=c, t=t, pb=pb: e.matmul(
                            ps[:, pb + 1, 0:256], lhsT=hT[:, c, t * 128:(t + 1) * 128], rhs=wkv_v[:, c, :],
                            start=(c == 0), stop=(c == 7)), [("hT", t), ("w", kk, 0)], [("ps", pb + 1)])
                    qk = ps[:, pb:pb + 2, :].rearrange("p a n -> p (a n)")[:, 0:640]
                    R.add("act", lambda e, qk=qk: e.activation(out=sq[:, :], in_=qk, func=AF.Square),
                          [("ps", pb), ("ps", pb + 1)], ["sq"])
                    R.add("dve", lambda e: e.tensor_reduce(out=sml[:, 0:10], in_=sq.rearrange("p (h d) -> p h d", d=64),
                                                           axis=AX.X, op=ALU.add), ["sq"], ["sml"])
                    R.add("act", lambda e: e.activation(out=sml[:, 16:26], in_=sml[:, 0:10], func=AF.Ln, bias=EPS,
                                                        scale=1.0 / 64), ["sml"], ["sml2"])
                    R.add("act", lambda e, b=b: e.activation(out=sml[:, 32 + b * 16:42 + b * 16], in_=sml[:, 16:26],
                                                             func=AF.Exp, scale=-0.5), ["sml2"], [("srs", b)])
                    R.add("dve", lambda e, b=b, qk=qk: e.tensor_tensor(
                        out=qn[b].rearrange("p (h d) -> p h d", d=64), in0=qk.rearrange("p (h d) -> p h d", d=64),
                        in1=sml[:, 32 + b * 16:42 + b * 16].unsqueeze(2).to_broadcast([128, 10, 64]), op=ALU.mult),
                        [("ps", pb), ("ps", pb + 1), ("srs", b)], [("qn", b)])
                    R.add("dve", lambda e, t=t, pb=pb: e.tensor_copy(
                        out=vsw[:, t, :, 0:64], in_=ps[:, pb + 1, 128:256].rearrange("p (g d) -> p g d", g=2)),
                        [("ps", pb + 1)], [("vsw", t)])
                    def part_b(t=t, b=b):
                        tp = ps[:, b, :].bitcast(BF16)
                        for c in range(5):
                            R.add("pe", lambda e, c=c, b=b, tp=tp: e.transpose(
                                out=tp[:, c * 128:(c + 1) * 128], in_=qn[b][:, c * 128:(c + 1) * 128],
                                identity=identb[:, :]), [("qn", b), "identb"], [("ps", b)])
                        R.add("dve", lambda e, t=t, tp=tp: e.tensor_scalar(
                            out=qTs[:, :, t * 128:(t + 1) * 128],
                            in0=tp[:, 0:512].rearrange("p (i n) -> p i n", i=4),
                            scalar1=cst[:, ccol(l, C_SWQ):ccol(l, C_SWQ) + 1], scalar2=0.125, op0=ALU.mult,
                            op1=ALU.mult), [("ps", b)], [("qTs", t)])
                        R.add("dve", lambda e, t=t, tp=tp: e.tensor_scalar(
                            out=kTs[:, t * 128:(t + 1) * 128], in0=tp[:, 512:640],
                            scalar1=cst[:, ccol(l, C_SWK):ccol(l, C_SWK) + 1], scalar2=None, op0=ALU.mult),
                            [("ps", b)], [("kTs", t)])
                    if swa_pend[0] is not None:
                        swa_pend[0]()
                    swa_pend[0] = part_b
                swa_pend[0]()
                it = 0
                att_pend = [None]
                for n in range(NT):
                    for g in range(2):
                        ob = 6 + ((n * 2 + g) % 2)
                        chunks = [(1, n)] if n == 0 else [(0, n - 1), (1, n)]
                        slots = []
                        for ci, (cc, kt) in enumerate(chunks):
                            sbk = it % 4
                            it += 1
                            slots.append(sbk)
                            zb = 2 + (sbk % 2)
                            R.add("pe", lambda e, kt=kt, n=n, g=g, zb=zb: e.matmul(
                                ps[:, zb, :], lhsT=kTs[g * 64:(g + 1) * 64, kt * 128:(kt + 1) * 128],
                                rhs=qTs[g * 64:(g + 1) * 64, :, n * 128:(n + 1) * 128], start=True, stop=True),
                                [("kTs", kt), ("qTs", n)], [("ps", zb)])
                            R.add("dve", lambda e, zb=zb, sbk=sbk, cc=cc, g=g: e.tensor_tensor(
                                out=sbf[sbk % 2].rearrange("p (i q) -> p i q", i=4),
                                in0=ps[:, zb, :].rearrange("p (i q) -> p i q", i=4),
                                in1=bia[:, cc, g * 4:(g + 1) * 4, :], op=ALU.add), [("ps", zb), "bia"],
                                [("sbf", sbk % 2)])
                            R.add("act", lambda e, sbk=sbk: e.activation(out=Pb[sbk][:, :], in_=sbf[sbk % 2][:, :],
                                                                         func=AF.Exp), [("sbf", sbk % 2)], [("P", sbk)])
                        def part_b(chunks=chunks, slots=slots, ob=ob, n=n, g=g):
                            for i in range(4):
                                for ci, (cc, kt) in enumerate(chunks):
                                    sbk = slots[ci]
                                    R.add("pe", lambda e, i=i, sbk=sbk, kt=kt, g=g, ob=ob, ci=ci, nck=len(chunks): e.matmul(
                                        ps[:, ob, i * 65:(i + 1) * 65], lhsT=Pb[sbk][:, i * 128:(i + 1) * 128],
                                        rhs=vsw[:, kt, g, :], start=(ci == 0), stop=(ci == nck - 1)),
                                        [("P", sbk), ("vsw", kt), "vsw1"], [("ps", ob)])
                            ov = ps[:, ob, 0:260].rearrange("p (i d) -> p i d", i=4)
                            osb = (n * 2 + g) % 2
                            R.add("dve", lambda e, ov=ov, g=g: e.tensor_tensor(
                                out=sml[:, 0:4], in0=ov[:, :, 64],
                                in1=cst[:, C_ESINK + l * 8 + g * 4:C_ESINK + l * 8 + g * 4 + 4], op=ALU.add),
                                [("ps", ob), "esink"], ["den"])
                            R.add("dve", lambda e: e.reciprocal(out=sml[:, 4:8], in_=sml[:, 0:4]), ["den"], ["rec"])
                            R.add("dve", lambda e, ov=ov, osb=osb: e.tensor_tensor(
                                out=osw[osb].rearrange("p (i d) -> p i d", i=4), in0=ov[:, :, 0:64],
                                in1=sml[:, 4:8].unsqueeze(2).to_broadcast([128, 4, 64]), op=ALU.mult),
                                [("ps", ob), "rec"], [("osw", osb)])
                            R.dma("sp", mixed_d[n * 128:(n + 1) * 128, 512 + g * 256:512 + (g + 1) * 256], osw[osb][:, :],
                                  [("osw", osb)], [("mixed", n)], key=("osw", osb))
                        if att_pend[0] is not None:
                            att_pend[0]()
                        att_pend[0] = part_b
                att_pend[0]()
            barrier()
            raw = [avb(6 * KB + i * 2 * KB, 1024) for i in range(2)]

            def raw_src(t):
                b = t % 2
                R.dma("sp", raw[b][:, :], mixed_d[t * 128:(t + 1) * 128, :], [("mixed", t)], [("raw", b)],
                      key=("raw", b))
                return raw[b]
            norm_T(raw_src, lambda t: ("raw", t % 2), NT, [(0, 512), (512, 1024)], ccol(l, C_GOUT),
                   lambda t: hT[:, :, t * 128:(t + 1) * 128], lambda t: ("hT", t))
            out_proj(wd_["w_out"][l], 8)
            barrier()

        def xattn(l, s):
            A0 = 6 * KB
            memt = [av(A0 + i * 4 * KB, 4 * KB) for i in range(2)]
            hmT = avb(A0 + 8 * KB, 2048).rearrange("p (c n) -> p c n", c=8)
            kTx = avb(A0 + 12 * KB, 1024).rearrange("p (h n) -> p h n", h=4)
            vx = avb(A0 + 14 * KB, 2 * 4 * 129 + 16)[:, 0:2 * 4 * 129].rearrange("p (m h d) -> p m h d", m=2, h=4)
            qTx = avb(A0 + 17 * KB, 8192).rearrange("p (h n) -> p h n", h=4)
            sq = av(A0 + 33 * KB, 2 * KB)
            qn = [avb(A0 + 35 * KB + i * KB, 512) for i in range(2)]
            Pb = [avb(A0 + 37 * KB + i * KB, 512) for i in range(4)]
            xor_ = [avb(A0 + 41 * KB + i * 4 * KB, 2048).rearrange("p (j n) -> p j n", j=4) for i in range(2)]
            sml = av(A0 + 49 * KB, 256)
            kwq = wslot()
            wq_v = wpool[:, kwq, :].rearrange("p (c n) -> p c n", c=8)
            R.dma("pool", wq_v, wd_["xattn_wq"][l].rearrange("(c p) n -> p c n", p=128), [], [("w", kwq, 0)],
                  key=("w", kwq, 0))
            kvs = []
            for half in range(2):
                k = wslot()
                wv = wpool[:, k, :].rearrange("p (c n) -> p c n", c=8)
                R.dma("pool", wv, wd_["xattn_wkv"][l][:, half * 512:(half + 1) * 512].rearrange(
                    "(c p) n -> p c n", p=128), [], [("w", k, 0)], key=("w", k, 0))
                kvs.append((k, wv))
            def mem_src(t):
                R.dma("sp", memt[t][:, :], mem_d[s, t * 128:(t + 1) * 128, :], [], [("memt", t)], key=("memt", t))
                return memt[t]
            norm_T(mem_src, lambda t: ("memt", t), 2, [(0, D)], ccol(l, C_GMEM),
                   lambda t: hmT[:, :, t * 128:(t + 1) * 128], lambda t: ("hmT", t))
            x_norm_T(ccol(l, C_GXAT))
            R.add("pool", lambda e: e.memset(vx[:, :, :, 128:129], 1.0), [], ["vx1"])

            def head_norm_T(pb, srcv, b, nh, gcolumn, scale, dst, dtok, stoks):
                R.add("act", lambda e: e.activation(out=sq[:, 0:nh * 128], in_=srcv, func=AF.Square), stoks, ["sq"])
                R.add("dve", lambda e: e.tensor_reduce(out=sml[:, 0:nh],
                                                       in_=sq[:, 0:nh * 128].rearrange("p (h d) -> p h d", d=128),
                                                       axis=AX.X, op=ALU.add), ["sq"], ["sml"])
                R.add("act", lambda e: e.activation(out=sml[:, 16:16 + nh], in_=sml[:, 0:nh], func=AF.Ln, bias=EPS,
                                                    scale=1.0 / 128), ["sml"], ["sml2"])
                R.add("act", lambda e: e.activation(out=sml[:, 32 + b * 8:32 + b * 8 + nh], in_=sml[:, 16:16 + nh],
                                                    func=AF.Exp, scale=-0.5), ["sml2"], [("srs", b)])
                R.add("dve", lambda e: e.tensor_tensor(
                    out=qn[b][:, 0:nh * 128].rearrange("p (h d) -> p h d", d=128),
                    in0=srcv.rearrange("p (h d) -> p h d", d=128),
                    in1=sml[:, 32 + b * 8:32 + b * 8 + nh].unsqueeze(2).to_broadcast([128, nh, 128]), op=ALU.mult),
                    stoks + [("srs", b)], [("qn", b)])
                def part_b():
                    tp = ps[:, b, :].bitcast(BF16)
                    for c in range(nh):
                        R.add("pe", lambda e, c=c: e.transpose(out=tp[:, c * 128:(c + 1) * 128],
                                                               in_=qn[b][:, c * 128:(c + 1) * 128],
                                                               identity=identb[:, :]),
                              [("qn", b), "identb"], [("ps", b)])
                    R.add("dve", lambda e: e.tensor_scalar(
                        out=dst, in0=tp[:, 0:nh * 128].rearrange("p (h n) -> p h n", h=nh),
                        scalar1=cst[:, gcolumn:gcolumn + 1], scalar2=scale, op0=ALU.mult, op1=ALU.mult),
                        [("ps", b)], [dtok])
                return part_b

            for mt in range(2):
                pb = 4 + mt * 2
                for half in range(2):
                    k, wv = kvs[half]
                    for c in range(8):
                        R.add("pe", lambda e, c=c, wv=wv, half=half, mt=mt, pb=pb: e.matmul(
                            ps[:, pb + half, :], lhsT=hmT[:, c, mt * 128:(mt + 1) * 128], rhs=wv[:, c, :],
                            start=(c == 0), stop=(c == 7)), [("hmT", mt), ("w", k, 0)], [("ps", pb + half)])
                head_norm_T(pb, ps[:, pb, :], mt, 4, ccol(l, C_XK), 1.0, kTx[:, :, mt * 128:(mt + 1) * 128],
                            ("kTx", mt), [("ps", pb)])()
                R.add("dve", lambda e, mt=mt, pb=pb: e.tensor_copy(
                    out=vx[:, mt, :, 0:128], in_=ps[:, pb + 1, :].rearrange("p (h d) -> p h d", h=4)),
                    [("ps", pb + 1)], [("vx", mt)])
            pend = None
            for t in range(NT):
                b = t % 2
                pb = 4 + b
                for c in range(8):
                    R.add("pe", lambda e, c=c, t=t, pb=pb: e.matmul(
                        ps[:, pb, :], lhsT=hT[:, c, t * 128:(t + 1) * 128], rhs=wq_v[:, c, :], start=(c == 0),
                        stop=(c == 7)), [("hT", t), ("w", kwq, 0)], [("ps", pb)])
                nxt = head_norm_T(pb, ps[:, pb, :], b, 4, ccol(l, C_XQ), 128 ** -0.5,
                                  qTx[:, :, t * 128:(t + 1) * 128], ("qTx", t), [("ps", pb)])
                if pend is not None:
                    pend()
                pend = nxt
            pend()
            it = 0
            xat_pend = [None]
            for qg in range(4):
                xb = qg % 2
                for h in range(4):
                    ob = 4 + (h % 2) * 2
                    slots = []
                    for kt in range(2):
                        sbk = it % 4
                        it += 1
                        slots.append(sbk)
                        zb = 2 + (sbk % 2)
                        R.add("pe", lambda e, h=h, kt=kt, qg=qg, zb=zb: e.matmul(
                            ps[:, zb, :], lhsT=kTx[:, h, kt * 128:(kt + 1) * 128],
                            rhs=qTx[:, h, qg * 512:(qg + 1) * 512], start=True, stop=True),
                            [("kTx", kt)] + [("qTx", qg * 4 + i) for i in range(4)], [("ps", zb)])
                        R.add("act", lambda e, zb=zb, sbk=sbk: e.activation(out=Pb[sbk][:, :], in_=ps[:, zb, :],
                                                                            func=AF.Exp), [("ps", zb)], [("P", sbk)])
                    def part_b(slots=slots, h=h, ob=ob, xb=xb):
                        for jj in range(4):
                            for kt in range(2):
                                sbk = slots[kt]
                                R.add("pe", lambda e, jj=jj, sbk=sbk, kt=kt, h=h, ob=ob: e.matmul(
                                    ps[:, ob + jj // 2, (jj % 2) * 129:(jj % 2 + 1) * 129],
                                    lhsT=Pb[sbk][:, jj * 128:(jj + 1) * 128], rhs=vx[:, kt, h, :], start=(kt == 0),
                                    stop=(kt == 1)), [("P", sbk), ("vx", kt), "vx1"], [("ps", ob + jj // 2)])
                        ov = ps[:, ob:ob + 2, 0:258].rearrange("p a (j d) -> p a j d", j=2)
                        R.add("dve", lambda e, ov=ov: e.reciprocal(out=sml[:, 48:52].rearrange("p (a j) -> p a j", a=2),
                                                                   in_=ov[:, :, :, 128]),
                              [("ps", ob), ("ps", ob + 1)], ["rec"])
                        for a2 in range(2):
                            R.add("dve", lambda e, ov=ov, a2=a2, h=h, xb=xb: e.tensor_tensor(
                                out=xor_[xb][:, a2 * 2:(a2 + 1) * 2, h * 128:(h + 1) * 128], in0=ov[:, a2, :, 0:128],
                                in1=sml[:, 48 + a2 * 2:50 + a2 * 2].unsqueeze(2).to_broadcast([128, 2, 128]), op=ALU.mult),
                                [("ps", ob + a2), "rec"], [("xor", xb)])
                    if xat_pend[0] is not None:
                        xat_pend[0]()
                    xat_pend[0] = part_b
                xat_pend[0]()
                xat_pend[0] = None
                for jj in range(4):
                    t = qg * 4 + jj
                    b = t % 2
                    tp = ps[:, b, :].bitcast(BF16)
                    for c in range(4):
                        R.add("pe", lambda e, c=c, jj=jj, xb=xb, tp=tp: e.transpose(
                            out=tp[:, c * 128:(c + 1) * 128], in_=xor_[xb][:, jj, c * 128:(c + 1) * 128],
                            identity=identb[:, :]), [("xor", xb), "identb"], [("ps", b)])
                    R.add("act", lambda e, t=t, tp=tp: e.activation(
                        out=hT[:, 0:4, t * 128:(t + 1) * 128], in_=tp[:, 0:512].rearrange("p (c n) -> p c n", c=4),
                        func=AF.Copy), [("ps", b)], [("hT", t)])
            out_proj(wd_["xattn_wo"][l], 4)
            barrier()

        def ffn(l):
            i = l // 2
            moe = (l % 2 == 1)
            A0 = 6 * KB
            actT = avb(A0, 8192).rearrange("p (j n) -> p j n", j=4)
            sg = [av(A0 + 16 * KB + k * 2 * KB, 2 * KB) for k in range(2)]
            hn32 = av(A0 + 20 * KB, 4 * KB)
            h32T = av(A0 + 24 * KB, 4 * KB).rearrange("p (c n) -> p c n", c=8)
            gates = av(A0 + 28 * KB, 512).rearrange("p (t e) -> p t e", t=16)
            sm = av(A0 + 29 * KB, 512)

            def router_tile(t, b, src, stok):
                R.add("dve", lambda e: e.tensor_scalar(out=hn32[:, :], in0=src, scalar1=stat[:, 16 + b * 4:17 + b * 4],
                                                       scalar2=None, op0=ALU.mult), [stok, ("rstd", b)], ["hn32"])
                tpf = ps[:, 2:4, :].rearrange("p a n -> p (a n)")
                for c in range(8):
                    R.add("pe", lambda e, c=c: e.transpose(out=tpf[:, c * 128:(c + 1) * 128],
                                                           in_=hn32[:, c * 128:(c + 1) * 128], identity=identf[:, :]),
                          ["hn32", "identf"], [("ps", 2), ("ps", 3)])
                gc = ccol(l, C_GFFN)
                R.add("dve", lambda e: e.tensor_tensor(
                    out=h32T, in0=tpf.rearrange("p (c n) -> p c n", c=8),
                    in1=cst[:, gc:gc + 8].unsqueeze(2).to_broadcast([128, 8, 128]), op=ALU.mult),
                    [("ps", 2), ("ps", 3)], ["h32T"])
                for c in range(8):
                    R.add("pe", lambda e, c=c: e.matmul(ps[:, 4, 0:NE], lhsT=h32T[:, c, :], rhs=rw[:, i, c, :],
                                                        start=(c == 0), stop=(c == 7)), ["h32T"], [("ps", 4)])
                lg, eq1, l2, eq2 = sm[:, 0:8], sm[:, 8:16], sm[:, 16:24], sm[:, 24:32]
                m1, m2, dd, ee, g1, g2 = (sm[:, 32:33], sm[:, 33:34], sm[:, 34:35], sm[:, 35:36], sm[:, 36:37],
                                          sm[:, 37:38])
                ga = sm[:, 40:48]
                dv = lambda fn, r, w: R.add("dve", fn, r, w)
                dv(lambda e: e.tensor_tensor(out=lg, in0=ps[:, 4, 0:NE], in1=cst[:, C_RB + i * 8:C_RB + i * 8 + 8],
                                             op=ALU.add), [("ps", 4)], ["r0"])
                dv(lambda e: e.tensor_reduce(out=m1, in_=lg, axis=AX.X, op=ALU.max), ["r0"], ["r1"])
                dv(lambda e: e.tensor_scalar(out=eq1, in0=lg, scalar1=m1, scalar2=None, op0=ALU.is_equal),
                   ["r0", "r1"], ["r2"])
                dv(lambda e: e.scalar_tensor_tensor(out=l2, in0=eq1, scalar=-1e30, in1=lg, op0=ALU.mult, op1=ALU.add),
                   ["r2", "r0"], ["r3"])
                dv(lambda e: e.tensor_reduce(out=m2, in_=l2, axis=AX.X, op=ALU.max), ["r3"], ["r4"])
                dv(lambda e: e.tensor_scalar(out=eq2, in0=l2, scalar1=m2, scalar2=None, op0=ALU.is_equal),
                   ["r3", "r4"], ["r5"])
                dv(lambda e: e.tensor_tensor(out=dd, in0=m2, in1=m1, op=ALU.subtract), ["r4", "r1"], ["r6"])
                R.add("act", lambda e: e.activation(out=ee, in_=dd, func=AF.Exp), ["r6"], ["r7"])
                dv(lambda e: e.tensor_scalar(out=g1, in0=ee, scalar1=1.0, scalar2=None, op0=ALU.add), ["r7"], ["r8"])
                dv(lambda e: e.reciprocal(out=g1, in_=g1), ["r8"], ["r8"])
                dv(lambda e: e.tensor_tensor(out=g2, in0=ee, in1=g1, op=ALU.mult), ["r7", "r8"], ["r9"])
                dv(lambda e: e.tensor_scalar(out=ga, in0=eq1, scalar1=g1, scalar2=None, op0=ALU.mult),
                   ["r2", "r8"], ["r10"])
                dv(lambda e: e.scalar_tensor_tensor(out=gates[:, t, :], in0=eq2, scalar=g2, in1=ga, op0=ALU.mult,
                                                    op1=ALU.add), ["r5", "r9", "r10"], ["gates"])

            x_norm_T(ccol(l, C_GFFN), router_tile if moe else None)

            def ffn_weights(ei):
                if moe:
                    return wd_["exp_w_gate"][i, ei], wd_["exp_w_up"][i, ei], wd_["exp_w_down"][i, ei]
                return wd_["dense_w_gate"][i], wd_["dense_w_up"][i], wd_["dense_w_down"][i]

            for ei in range(NE if moe else 1):
                wg_d, wu_d, wdn_d = ffn_weights(ei)
                for fg in range(7):
                    f0 = fg * 512
                    kg, ku, kd = wslot(), wslot(), wslot()
                    wgv = wpool[:, kg, :].rearrange("p (c n) -> p c n", c=8)
                    wuv = wpool[:, ku, :].rearrange("p (c n) -> p c n", c=8)
                    wdv = wpool[:, kd, :].rearrange("p (j n) -> p j n", j=4)
                    R.dma("pool", wgv, wg_d[:, f0:f0 + 512].rearrange("(c p) f -> p c f", p=128), [], [("w", kg, 0)],
                          key=("w", kg, 0))
                    R.dma("pool", wuv, wu_d[:, f0:f0 + 512].rearrange("(c p) f -> p c f", p=128), [], [("w", ku, 0)],
                          key=("w", ku, 0))
                    R.dma("pool", wdv, wdn_d[f0:f0 + 512, :].rearrange("(j p) d -> p j d", p=128), [], [("w", kd, 0)],
                          key=("w", kd, 0))
                    kk = 0
                    for tg in range(4):
                        hr = [("hT", tg * 4 + q) for q in range(4)]
                        for j in range(4):
                            pb = (kk % 2) * 2
                            sb_ = kk % 2
                            kk += 1
                            for c in range(8):
                                R.add("pe", lambda e, c=c, j=j, tg=tg, pb=pb, wgv=wgv: e.matmul(
                                    ps[:, pb, :], lhsT=wgv[:, c, j * 128:(j + 1) * 128],
                                    rhs=hT[:, c, tg * 512:(tg + 1) * 512], start=(c == 0), stop=(c == 7)),
                                    hr + [("w", kg, 0)], [("ps", pb)])
                            for c in range(8):
                                R.add("pe", lambda e, c=c, j=j, tg=tg, pb=pb, wuv=wuv: e.matmul(
                                    ps[:, pb + 1, :], lhsT=wuv[:, c, j * 128:(j + 1) * 128],
                                    rhs=hT[:, c, tg * 512:(tg + 1) * 512], start=(c == 0), stop=(c == 7)),
                                    hr + [("w", ku, 0)], [("ps", pb + 1)])
                            R.add("act", lambda e, pb=pb, sb_=sb_: e.activation(out=sg[sb_][:, :], in_=ps[:, pb, :],
                                                                                func=AF.Silu),
                                  [("ps", pb)], [("sg", sb_)])
                            R.add("dve", lambda e, pb=pb, sb_=sb_, j=j, tg=tg: e.tensor_tensor(
                                out=actT[:, j, tg * 512:(tg + 1) * 512], in0=sg[sb_][:, :], in1=ps[:, pb + 1, :],
                                op=ALU.mult), [("sg", sb_), ("ps", pb + 1)], [("actT", tg)])
                    for t in range(NT):
                        pb = 4 + (t % 2) * 2
                        for half in range(2):
                            for j in range(4):
                                R.add("pe", lambda e, t=t, j=j, half=half, pb=pb, wdv=wdv: e.matmul(
                                    ps[:, pb + half, :], lhsT=actT[:, j, t * 128:(t + 1) * 128],
                                    rhs=wdv[:, j, half * 512:(half + 1) * 512], start=(j == 0), stop=(j == 3)),
                                    [("actT", t // 4), ("w", kd, 0)], [("ps", pb + half)])
                        add_to_x(t, pb, gates[:, t, ei:ei + 1] if moe else None)
            barrier()

        for s in range(nseq):
            for t in range(NT):
                R.dma("sp", x[:, t, :], x_d[s, t * 128:(t + 1) * 128, :], [], [("x", t)], key=("x", t))
            for l in layers:
                if "mix" in phases or "sb" in phases or "swa" in phases:
                    mixer(l)
                if "xat" in phases:
                    xattn(l, s)
                if "ffn" in phases:
                    ffn(l)
            for t in range(NT):
                R.dma("sp", y_d[s, t * 128:(t + 1) * 128, :], x[:, t, :], [("x", t)], [("y", s, t)], key=("x", t))
        R.add("sp", None, [("y", s, t) for s in range(nseq) for t in range(NT)], [])
        nsem = R.emit(nc)
        build_program.info = (R.nops, nsem)
    return nc


def _t5_buckets(dist):
    n = np.maximum(dist, 0)
    max_exact = 16
    large = max_exact + (np.log(np.maximum(n, 1) / max_exact) / np.log(128 / max_exact) * (32 - max_exact)).astype(
        np.int32)
    large = np.minimum(large, 31)
    return np.where(n < max_exact, n, large).astype(np.int32)


def prep_shared(inputs):
    rel_bias = np.asarray(inputs["rel_bias"], dtype=np.float32)
    dist = 128 + np.arange(128)[:, None] - np.arange(256)[None, :]
    bt = rel_bias[_t5_buckets(dist)]
    biast = np.ascontiguousarray(np.transpose(bt, (1, 2, 0)))
    w_in = np.array(inputs["w_in"], dtype=np.float32, copy=True)
    q = w_in[:, :, 1536:2048].reshape(DEPTH, D, 2, 4, 64)
    w_in[:, :, 1536:2048] = np.transpose(q, (0, 1, 3, 2, 4)).reshape(DEPTH, D, 512)
    shared = {n: np.ascontiguousarray(np.asarray(inputs[n], dtype=np.float32)) for n in W_NAMES if n != "w_in"}
    shared["w_in"] = w_in
    shared["swa_bias_t"] = biast
    return shared


_CACHE = {}


def kernel(**inputs):
    n_cores = 8
    x = np.asarray(inputs["x"], dtype=np.float32)
    mem = np.asarray(inputs["mem"], dtype=np.float32)
    B = x.shape[0]
    nseq = B // n_cores
    if "nc" not in _CACHE:
        _CACHE["nc"] = build_program(nseq=nseq)
    nc = _CACHE["nc"]
    shared = prep_shared(inputs)
    in_maps = []
    for c in range(n_cores):
        m = dict(shared)
        m["x"] = np.ascontiguousarray(x[c * nseq:(c + 1) * nseq])
        m["mem"] = np.ascontiguousarray(mem[c * nseq:(c + 1) * nseq])
        in_maps.append(m)
    res = run_bass_kernel_spmd(nc, in_maps, core_ids=list(range(n_cores)))
    return np.concatenate([r["y"] for r in res.results], axis=0)
```
